# Optimizing a Trainium2 kernel written in Bass

```python
import math
import jax, jax.numpy as jnp
from jax import lax
import numpy as np

D_MODEL = 4096
BATCH = 4
SEQ = 2048
DEPTH = 1

HEAD_DIM = 128
FOX_HEADS = 16
DSA_HEADS = 16
FOX_WIDTH = FOX_HEADS * HEAD_DIM
DSA_WIDTH = DSA_HEADS * HEAD_DIM
MIX_WIDTH = FOX_WIDTH + DSA_WIDTH
Q_LORA = 1536
KV_LORA = 512
IDX_HEADS = 32
IDX_DIM = 128
TOPK_MAX = 256
Q_BLOCK = 128
N_BUCKETS = 32
MAX_DISTANCE = 128
N_EXPERTS = 32
TOP_K = 4
D_EXPERT = 1536
SWIGLU_LIMIT = 7.0
SWIGLU_ALPHA = 1.702
EXPERT_BLOCK = 128
ALPHA_RES = (2 * DEPTH) ** 0.25
BETA_INIT = (8 * DEPTH) ** -0.25
_IN_SIZES = (FOX_WIDTH, FOX_WIDTH, FOX_WIDTH, FOX_HEADS, Q_LORA, KV_LORA, IDX_DIM, IDX_HEADS)
IN_COLS = sum(_IN_SIZES)
IN_SPLITS = tuple(int(v) for v in np.cumsum(_IN_SIZES)[:-1])

kernel_name = 'hybrid_fox_dsa_moe_block'


def layer_norm(x, g, b, eps=1e-5):
    x32 = x.astype(jnp.float32)
    mu = jnp.mean(x32, axis=-1, keepdims=True)
    var = jnp.mean(jnp.square(x32 - mu), axis=-1, keepdims=True)
    y = (x32 - mu) * lax.rsqrt(var + eps) * g.astype(jnp.float32) + b.astype(jnp.float32)
    return y.astype(x.dtype)


def rms_norm(x, g, eps=1e-6):
    x32 = x.astype(jnp.float32)
    y = x32 * lax.rsqrt(jnp.mean(jnp.square(x32), axis=-1, keepdims=True) + eps) * g.astype(jnp.float32)
    return y.astype(x.dtype)


def to_blocks(a):
    b, s = a.shape[:2]
    a = a.reshape((b, s // Q_BLOCK, Q_BLOCK) + a.shape[2:])
    return jnp.moveaxis(a, 1, 0)


def from_blocks(a):
    a = jnp.moveaxis(a, 0, 1)
    return a.reshape((a.shape[0], a.shape[1] * a.shape[2]) + a.shape[3:])


def t5_bucket(dist):
    max_exact = N_BUCKETS // 2
    d = jnp.maximum(dist.astype(jnp.float32), 1.0)
    large = max_exact + (jnp.log(d / max_exact) / math.log(MAX_DISTANCE / max_exact)
                         * (N_BUCKETS - max_exact)).astype(jnp.int32)
    large = jnp.minimum(large, N_BUCKETS - 1)
    return jnp.where(dist < max_exact, dist, large)


def fox_attention(q, k, v, log_f):
    seq = q.shape[1]
    cum = jnp.cumsum(log_f, axis=1)
    cum_keys = jnp.swapaxes(cum, 1, 2)
    pos_k = jnp.arange(seq)
    scale = HEAD_DIM ** -0.5

    def block(args):
        i, q_i, cum_i = args
        pos_q = i * Q_BLOCK + jnp.arange(Q_BLOCK)
        s = jnp.einsum('bthd,bshd->bhts', q_i, k).astype(jnp.float32) * scale
        s = s + jnp.swapaxes(cum_i, 1, 2)[..., None] - cum_keys[:, :, None, :]
        s = jnp.where(pos_k[None, :] <= pos_q[:, None], s, -jnp.inf)
        p = jax.nn.softmax(s, axis=-1).astype(v.dtype)
        return jnp.einsum('bhts,bshd->bthd', p, v)

    nb = seq // Q_BLOCK
    out = lax.map(block, (jnp.arange(nb), to_blocks(q), to_blocks(cum)))
    return from_blocks(out)


def dsa_attention(q, c_kv, q_idx, k_idx, w_idx, w_uk, w_uv, rel_bias, k_sel):
    seq = q.shape[1]
    pos_k = jnp.arange(seq)
    scale = HEAD_DIM ** -0.5

    def block(args):
        i, q_i, qi_i, wi_i = args
        pos_q = i * Q_BLOCK + jnp.arange(Q_BLOCK)
        causal = pos_k[None, :] <= pos_q[:, None]
        rel = jax.nn.relu(jnp.einsum('bthd,bsd->bths', qi_i, k_idx).astype(jnp.float32))
        score = jnp.einsum('bths,bth->bts', rel, wi_i.astype(jnp.float32))
        score = jnp.where(causal[None], score, -jnp.inf)
        _, idx = lax.top_k(score, k_sel)
        valid = idx <= pos_q[None, :, None]
        ckv_sel = jax.vmap(lambda c_b, j_b: c_b[j_b])(c_kv, idx)
        q_abs = jnp.einsum('bthd,rhd->bthr', q_i, w_uk)
        s = jnp.einsum('bthr,btkr->bhtk', q_abs, ckv_sel).astype(jnp.float32) * scale
        dist = jnp.maximum(pos_q[None, :, None] - idx, 0)
        bias = rel_bias[t5_bucket(dist)].astype(jnp.float32)
        s = s + jnp.transpose(bias, (0, 3, 1, 2))
        s = jnp.where(valid[:, None], s, -jnp.inf)
        p = jax.nn.softmax(s, axis=-1).astype(c_kv.dtype)
        lat = jnp.einsum('bhtk,btkr->bthr', p, ckv_sel)
        return jnp.einsum('bthr,rhd->bthd', lat, w_uv)

    nb = seq // Q_BLOCK
    out = lax.map(block, (jnp.arange(nb), to_blocks(q), to_blocks(q_idx), to_blocks(w_idx)))
    return from_blocks(out)


def hybrid_mixer(u, w_in, b_forget, q_norm_g, kv_norm_g, kidx_ln_g, kidx_ln_b,
                 w_uq, w_uk, w_uv, w_iq, fox_out_g, dsa_out_g, w_o, rel_bias):
    bsz, seq, _ = u.shape
    proj = u @ w_in
    q_f, k_f, v_f, f_log, c_q, c_kv, k_idx, w_idx = jnp.split(proj, IN_SPLITS, axis=-1)
    log_f = jax.nn.log_sigmoid((f_log + b_forget).astype(jnp.float32))
    shp = (bsz, seq, FOX_HEADS, HEAD_DIM)
    fox = fox_attention(q_f.reshape(shp), k_f.reshape(shp), v_f.reshape(shp), log_f)
    fox = fox.reshape(bsz, seq, FOX_WIDTH)
    c_q = rms_norm(c_q, q_norm_g)
    c_kv = rms_norm(c_kv, kv_norm_g)
    q_d = (c_q @ w_uq).reshape(bsz, seq, DSA_HEADS, HEAD_DIM)
    q_i = (c_q @ w_iq).reshape(bsz, seq, IDX_HEADS, IDX_DIM)
    k_i = layer_norm(k_idx, kidx_ln_g, kidx_ln_b)
    w_i = w_idx * (IDX_HEADS ** -0.5 * IDX_DIM ** -0.5)
    k_sel = min(TOPK_MAX, seq // 4)
    dsa = dsa_attention(q_d, c_kv, q_i, k_i, w_i, w_uk, w_uv, rel_bias, k_sel)
    dsa = dsa.reshape(bsz, seq, DSA_WIDTH)
    merged = jnp.concatenate([rms_norm(fox, fox_out_g), rms_norm(dsa, dsa_out_g)], axis=-1)
    return merged @ w_o


def clamped_swiglu(h):
    gate, up = h[..., :D_EXPERT], h[..., D_EXPERT:]
    gate = jnp.minimum(gate, SWIGLU_LIMIT)
    up = jnp.clip(up, -SWIGLU_LIMIT, SWIGLU_LIMIT)
    return (up + 1.0) * gate * jax.nn.sigmoid(SWIGLU_ALPHA * gate)


def moe_ffn(u, w_router, b_router, w1, b1, w2, b2):
    bsz, seq, d = u.shape
    n_tok = bsz * seq
    xf = u.reshape(n_tok, d)
    logits = (xf @ w_router + b_router).astype(jnp.float32)
    top_val, top_idx = lax.top_k(logits, TOP_K)
    gates = jax.nn.softmax(top_val, axis=-1)
    n_assign = n_tok * TOP_K
    e_flat = top_idx.reshape(n_assign)
    tok_flat = jnp.repeat(jnp.arange(n_tok, dtype=jnp.int32), TOP_K)
    g_flat = gates.reshape(n_assign)
    order = jnp.argsort(e_flat)
    e_s, tok_s, g_s = e_flat[order], tok_flat[order], g_flat[order]
    counts = jnp.zeros((N_EXPERTS,), jnp.int32).at[e_flat].add(1)
    starts = jnp.cumsum(counts) - counts
    padded = (counts + EXPERT_BLOCK - 1) // EXPERT_BLOCK * EXPERT_BLOCK
    pad_ends = jnp.cumsum(padded)
    pad_starts = pad_ends - padded
    dest = pad_starts[e_s] + (jnp.arange(n_assign, dtype=jnp.int32) - starts[e_s])
    n_blocks = -(-n_assign // EXPERT_BLOCK) + N_EXPERTS
    n_slots = n_blocks * EXPERT_BLOCK
    slot_tok = jnp.full((n_slots,), n_tok, jnp.int32).at[dest].set(tok_s)
    slot_gate = jnp.zeros((n_slots,), jnp.float32).at[dest].set(g_s)
    blk_start = jnp.arange(n_blocks, dtype=jnp.int32) * EXPERT_BLOCK
    blk_expert = jnp.minimum(jnp.searchsorted(pad_ends, blk_start, side='right'), N_EXPERTS - 1)
    x_pad = jnp.concatenate([xf, jnp.zeros((1, d), xf.dtype)], axis=0)

    def block(args):
        toks, e = args
        h = x_pad[toks] @ w1[e] + b1[e]
        return clamped_swiglu(h) @ w2[e] + b2[e]

    out = lax.map(block, (slot_tok.reshape(n_blocks, EXPERT_BLOCK), blk_expert))
    contrib = out.reshape(n_slots, d) * slot_gate[:, None].astype(out.dtype)
    y = jnp.zeros((n_tok + 1, d), out.dtype).at[slot_tok].add(contrib)
    return y[:n_tok].reshape(bsz, seq, d)


def setup_inputs(seed: int = 0) -> dict:
    key = jax.random.key(seed)
    ks = jax.random.split(key, 32)
    L, D = DEPTH, D_MODEL

    def nrm(k, shape, std):
        return jax.random.normal(k, shape, jnp.float32) * std

    def gain(k, shape):
        return 1.0 + nrm(k, shape, 0.02)

    col_scale = jnp.ones((IN_COLS,), jnp.float32).at[2 * FOX_WIDTH:3 * FOX_WIDTH].set(BETA_INIT)
    return {
        'x': nrm(ks[0], (BATCH, SEQ, D), 1.0),
        'c': nrm(ks[1], (BATCH, D), 1.0),
        'w_ada': nrm(ks[2], (L, D, 6 * D), 0.5 * D ** -0.5),
        'b_ada': nrm(ks[3], (L, 6 * D), 0.02),
        'w_in': nrm(ks[4], (L, D, IN_COLS), D ** -0.5) * col_scale,
        'b_forget': jax.random.uniform(ks[5], (L, FOX_HEADS), jnp.float32, 1.0, 6.0),
        'q_norm_g': gain(ks[6], (L, Q_LORA)),
        'kv_norm_g': gain(ks[7], (L, KV_LORA)),
        'kidx_ln_g': gain(ks[8], (L, IDX_DIM)),
        'kidx_ln_b': nrm(ks[9], (L, IDX_DIM), 0.02),
        'w_uq': nrm(ks[10], (L, Q_LORA, DSA_WIDTH), Q_LORA ** -0.5),
        'w_uk': nrm(ks[11], (L, KV_LORA, DSA_HEADS, HEAD_DIM), KV_LORA ** -0.5),
        'w_uv': nrm(ks[12], (L, KV_LORA, DSA_HEADS, HEAD_DIM), BETA_INIT * KV_LORA ** -0.5),
        'w_iq': nrm(ks[13], (L, Q_LORA, IDX_HEADS * IDX_DIM), Q_LORA ** -0.5),
        'fox_out_g': gain(ks[14], (L, FOX_WIDTH)),
        'dsa_out_g': gain(ks[15], (L, DSA_WIDTH)),
        'w_o': nrm(ks[16], (L, MIX_WIDTH, D), BETA_INIT * MIX_WIDTH ** -0.5),
        'ln1_g': gain(ks[17], (L, D)),
        'ln1_b': nrm(ks[18], (L, D), 0.02),
        'w_router': nrm(ks[19], (L, D, N_EXPERTS), D ** -0.5),
        'b_router': nrm(ks[20], (L, N_EXPERTS), 0.01),
        'w1': nrm(ks[21], (L, N_EXPERTS, D, 2 * D_EXPERT), BETA_INIT * D ** -0.5),
        'b1': nrm(ks[22], (L, N_EXPERTS, 2 * D_EXPERT), 0.02),
        'w2': nrm(ks[23], (L, N_EXPERTS, D_EXPERT, D), BETA_INIT * D_EXPERT ** -0.5),
        'b2': nrm(ks[24], (L, N_EXPERTS, D), 0.02),
        'ln2_g': gain(ks[25], (L, D)),
        'ln2_b': nrm(ks[26], (L, D), 0.02),
        'rel_bias': nrm(ks[27], (N_BUCKETS, DSA_HEADS), 0.5),
    }


def reference(x, c, w_ada, b_ada, w_in, b_forget, q_norm_g, kv_norm_g, kidx_ln_g, kidx_ln_b,
              w_uq, w_uk, w_uv, w_iq, fox_out_g, dsa_out_g, w_o, ln1_g, ln1_b,
              w_router, b_router, w1, b1, w2, b2, ln2_g, ln2_b, rel_bias):
    for l in range(DEPTH):
        mod = jax.nn.silu(c) @ w_ada[l] + b_ada[l]
        sh_a, sc_a, g_a, sh_m, sc_m, g_m = jnp.split(mod[:, None, :], 6, axis=-1)
        u = x * (1.0 + sc_a) + sh_a
        mix = hybrid_mixer(u, w_in[l], b_forget[l], q_norm_g[l], kv_norm_g[l], kidx_ln_g[l],
                           kidx_ln_b[l], w_uq[l], w_uk[l], w_uv[l], w_iq[l], fox_out_g[l],
                           dsa_out_g[l], w_o[l], rel_bias)
        x = layer_norm(ALPHA_RES * x + g_a * mix, ln1_g[l], ln1_b[l])
        u = x * (1.0 + sc_m) + sh_m
        ffn = moe_ffn(u, w_router[l], b_router[l], w1[l], b1[l], w2[l], b2[l])
        x = layer_norm(ALPHA_RES * x + g_m * ffn, ln2_g[l], ln2_b[l])
    return x
```

```python
import contextlib
import numpy as np
import concourse.bass as bass
import concourse.mybir as mybir
from concourse.bass_utils import run_bass_kernel_spmd

F32 = mybir.dt.float32
BF16 = mybir.dt.bfloat16
I32 = mybir.dt.int32
U32 = mybir.dt.uint32
ALU = mybir.AluOpType
AF = mybir.ActivationFunctionType
AX = mybir.AxisListType

QUEUES = ("pe", "act", "dve", "pool", "sp")


class Buf:
    __slots__ = ("name", "last_w", "readers")

    def __init__(self, name="b"):
        self.name = name
        self.last_w = None
        self.readers = []


def bufs(n, name="b"):
    return [Buf(f"{name}{i}") for i in range(n)]


class Op:
    __slots__ = ("idx", "q", "fn", "deps", "is_dma", "chan", "signal", "ticket", "sem")

    def __init__(self, idx, q, fn, is_dma, chan):
        self.idx = idx
        self.q = q
        self.fn = fn
        self.deps = []
        self.is_dma = is_dma
        self.chan = chan
        self.signal = False
        self.ticket = None
        self.sem = None


class Prog:
    def __init__(self, nc):
        self.nc = nc
        self.ops = []
        self.chan_last = {}
        self.chan_map = {}
        self.last_op = {q: None for q in QUEUES}
        self.stack = contextlib.ExitStack()
        self.nsb = 0

    def sbuf(self, st, shape, dtype, name=None):
        self.nsb += 1
        return st.enter_context(self.nc.sbuf_tensor(f"{name or 'sb'}_{self.nsb}", list(shape), dtype))

    def op(self, q, fn, reads=(), writes=(), dma=False, chan=None, extra=()):
        o = Op(len(self.ops), q, fn, dma, chan)
        deps = {}
        for b in reads:
            if b.last_w is not None:
                deps[b.last_w.idx] = b.last_w
        for b in writes:
            if b.last_w is not None:
                deps[b.last_w.idx] = b.last_w
            for r in b.readers:
                deps[r.idx] = r
        for d in extra:
            if d is not None:
                deps[d.idx] = d
        if dma:
            chan = self.chan_map.setdefault(chan, f"C{len(self.chan_map)}")
            o.chan = chan
            prev = self.chan_last.get(chan)
            if prev is not None:
                deps[prev.idx] = prev
            self.chan_last[chan] = o
        for d in deps.values():
            if (not d.is_dma) and (not dma) and d.q == q and q == "pe":
                continue
            o.deps.append(d)
            d.signal = True
        rk = ("dma", chan) if dma else q
        for b in reads:
            b.readers = [r for r in b.readers if (("dma", r.chan) if r.is_dma else r.q) != rk] + [o]
        for b in writes:
            b.last_w = o
            b.readers = []
        if not dma:
            self.last_op[q] = o
        self.ops.append(o)
        return o

    def dma(self, q, out, in_, reads, writes, chan, **kw):
        return self.op(q, lambda e: e.dma_start(out=out, in_=in_, **kw), reads, writes, dma=True, chan=chan)

    def barrier(self):
        pend = [d for d in self.chan_last.values()] + [d for d in self.last_op.values() if d is not None]
        for q in QUEUES:
            self.op(q, lambda e: e.nop(), extra=pend)
        self.chan_map = {}

    def emit(self):
        nc = self.nc
        st = self.stack
        qsem = {q: st.enter_context(nc.semaphore(f"q_{q}")) for q in QUEUES}
        chans = {}
        for o in self.ops:
            if o.is_dma and o.chan not in chans:
                chans[o.chan] = st.enter_context(nc.semaphore(f"c_{len(chans)}"))
        qcount = {q: 0 for q in QUEUES}
        ccount = {c: 0 for c in chans}
        for o in self.ops:
            if o.is_dma:
                ccount[o.chan] += 16
                o.sem = chans[o.chan]
                o.ticket = ccount[o.chan]
            elif o.signal:
                qcount[o.q] += 1
                o.sem = qsem[o.q]
                o.ticket = qcount[o.q]
        self.stats = dict(qcount=qcount, nchan=len(chans), maxc=max(list(ccount.values()) + [0]), nops=len(self.ops))
        byq = {q: [o for o in self.ops if o.q == q] for q in QUEUES}
        blk = st.enter_context(nc.Block())

        def run(q):
            def body(e):
                waited = {}
                for o in byq[q]:
                    need = {}
                    for d in o.deps:
                        k = id(d.sem)
                        if k not in need or need[k][1] < d.ticket:
                            need[k] = (d.sem, d.ticket)
                    for k, (s, v) in need.items():
                        if waited.get(k, 0) >= v:
                            continue
                        e.wait_ge(s, v)
                        waited[k] = v
                    try:
                        ins = o.fn(e)
                    except Exception:
                        print("EMIT FAIL op", o.idx, o.q, o.is_dma, o.chan, "deps", [(d.idx, d.q, d.ticket) for d in o.deps], flush=True)
                        raise
                    if o.is_dma:
                        ins.then_inc(o.sem, 16)
                    elif o.signal:
                        ins.then_inc(o.sem, 1)
            return body

        blk.tensor(run("pe"))
        blk.scalar(run("act"))
        blk.vector(run("dve"))
        blk.gpsimd(run("pool"))
        blk.sync(run("sp"))


D = 4096
KC = 32
S = 2048
T = 1024
NQT = 8
NE = 32
CAP = 256
ALPHA = 2.0 ** 0.25
SCALE = 128.0 ** -0.5
C_QF, C_KF, C_VF, C_F, C_CQ, C_CKV, C_KI, C_WI = 0, 2048, 4096, 6144, 6160, 7696, 8208, 8336
NEGBIG = -3.0e5
ZROW = NE * CAP


def build(stop_after=99, dbg=False, ne=NE):
    nc = bass.Bass("TRN2", target_bir_lowering=False)
    P = Prog(nc)
    G = P.stack

    def din(name, shape, dt=F32):
        return nc.dram_tensor(name, list(shape), dt, kind="ExternalInput").ap()

    def dscr(name, shape, dt):
        if dbg and name in dbg:
            return nc.dram_tensor(name, list(shape), dt, kind="ExternalOutput").ap()
        return nc.dram_tensor(name, list(shape), dt).ap()

    _specs = {
        "x_all": [S, D],
        "x_own": [T, D],
        "cT": [128, KC],
        "w_ada": [D, 6 * D],
        "b_ada": [1, 6 * D],
        "w_in": [D, 8368],
        "b_forget": [1, 16],
        "qn_g": [128, 12],
        "kvn_g": [128, 4],
        "ki_gb": [128, 2],
        "w_uq": [1536, 2048],
        "w_uk": [512, 2048],
        "w_uv": [512, 2048],
        "w_iq": [1536, 4096],
        "mg_g": [128, KC],
        "w_o": [D, D],
        "ln1_g": [1, D],
        "ln1_b": [1, D],
        "w_router": [D, NE],
        "b_router": [1, NE],
        "w1": [ne, D, 3072],
        "b1": [ne, 3072],
        "w2": [ne, 1536, D],
        "b2": [ne, D],
        "ln2_g": [1, D],
        "ln2_b": [1, D],
        "biasG": [16, 128, 2304],
        "ident": [128, 128],
        "esel": [80, 2048],
        "maskc": [128, 256],
        "triu": [128, 128],
        "iota": [128, 256],
        "tidx": [128, 16],
    }
    _ins = {}

    def I(name):
        if name not in _ins:
            _ins[name] = din(name, _specs[name])
        return _ins[name]

    y_out = nc.dram_tensor("y", [T, D], F32, kind="ExternalOutput").ap()

    mod_d = dscr("mod_d", [1, 6 * D], F32)
    kT_d = dscr("kT_d", [16, 128, S], BF16)
    v_d = dscr("v_d", [S, 2048], BF16)
    flog_d = dscr("flog_d", [S, 16], F32)
    ckvT_d = dscr("ckvT_d", [4, 128, S], F32)
    kidxT_d = dscr("kidxT_d", [128, S], F32)
    qT_d = dscr("qT_d", [16, 128, T], BF16)
    cqT_d = dscr("cqT_d", [12, 128, T], F32)
    widx_d = dscr("widx_d", [T, 32], F32)
    kdT_d = dscr("kdT_d", [16, 128, S], BF16)
    vd_d = dscr("vd_d", [S, 2048], BF16)
    qdT_d = dscr("qdT_d", [16, 128, T], BF16)
    qiT_d = dscr("qiT_d", [32, 128, T], BF16)
    sel_d = dscr("sel_d", [NQT, 128, S], BF16)
    r_d = dscr("r_d", [T, D], F32)
    x1_d = dscr("x1_d", [T, D], F32)
    u2_d = dscr("u2_d", [T, D], BF16)
    ys_d = dscr("ys_d", [NE * CAP + 128, D], F32)
    attn_d = dscr("attn_d", [T, D], BF16)

    PS = G.enter_context(nc.psum_tensor("PS", [128, 4096], F32))
    pb = bufs(8, "pb")
    PSB = PS[:].bitcast(BF16)

    def ps(b0, nb=1, rows=128, cols=None):
        c = cols if cols is not None else nb * 512
        return PS[0:rows, b0 * 512:b0 * 512 + c]

    ident_f = P.sbuf(G, [128, 128], F32, "identf")
    ident_b = P.sbuf(G, [128, 128], BF16, "identb")
    ones_f = P.sbuf(G, [128, 128], F32, "onesf")
    ones_b = P.sbuf(G, [128, 128], BF16, "onesb")
    modT = P.sbuf(G, [128, 192], F32, "modT")
    scA1 = P.sbuf(G, [128, KC], F32, "scA1")
    CS = P.sbuf(G, [80, S], BF16, "CS")
    kiT = P.sbuf(G, [128, S], BF16, "kiT")
    Esel = P.sbuf(G, [80, 2048], BF16, "Esel")
    maskb = P.sbuf(G, [128, 256], BF16, "maskb")
    maskbig = P.sbuf(G, [128, 256], F32, "maskbig")
    cbuf = Buf("consts")

    def MM(out, lhsT, rhs, start, stop, R, W, **kw):
        return P.op("pe", lambda e: e.matmul(out, lhsT=lhsT, rhs=rhs, start=start, stop=stop, **kw), R, W)

    def TR(out, in_, idn, R, W):
        return P.op("pe", lambda e: e.transpose(out, in_, idn), R, W)

    def ACT(out, in_, func, R, W, **kw):
        return P.op("act", lambda e: e.activation(out=out, in_=in_, func=func, **kw), R, W)

    def VOP(q, meth, R, W, *a, **kw):
        return P.op(q, lambda e: getattr(e, meth)(*a, **kw), R, W)

    def cast_load(dst, src, R, W, chan):
        return P.dma("pool", dst, src, R, W, chan)

    P.dma("sp", ident_f[:], I("ident"), [], [cbuf], "c0")
    VOP("dve", "tensor_copy", [cbuf], [cbuf], out=ident_b[:], in_=ident_f[:])
    VOP("dve", "memset", [], [cbuf], ones_f[:], 1.0)
    VOP("dve", "memset", [], [cbuf], ones_b[:], 1.0)
    cast_load(Esel[:], I("esel"), [], [cbuf], "c1")
    with contextlib.ExitStack() as s0:
        mtmp = P.sbuf(s0, [128, 256], F32, "mtmp")
        P.dma("sp", mtmp[:], I("maskc"), [], [cbuf], "c0")
        VOP("dve", "tensor_scalar", [cbuf], [cbuf], out=maskb[:], in0=mtmp[:], scalar1=NEGBIG, scalar2=None, op0=ALU.mult)
        VOP("dve", "tensor_scalar", [cbuf], [cbuf], out=maskbig[:], in0=mtmp[:], scalar1=-1.0e30, scalar2=None, op0=ALU.mult)
        P.barrier()

    sil = P.sbuf(G, [128, KC], BF16, "sil")
    b_sil = Buf("sil")
    wv_ada = I("w_ada").rearrange("(kc p) n -> p kc n", p=128)
    N_S0 = 24

    def mod_issue(g, wt_t, wt_b, brow_t, brow_b, tag):
        cast_load(wt_t[:], wv_ada[:, :, g * 512:(g + 1) * 512], [], [wt_b], f"ada{tag}")
        P.dma("sp", brow_t[:], I("b_ada")[:, g * 512:(g + 1) * 512], [], [brow_b], f"abr{tag}")

    def mod_compute(g, wt_t, wt_b, brow_t, brow_b, row_t, row_b, bank, tag, transpose):
        for kc in range(KC):
            MM(ps(bank, rows=1), sil[:, kc:kc + 1], wt_t[:, kc, :], kc == 0, kc == KC - 1, [b_sil, wt_b], [pb[bank]])
        VOP("dve", "tensor_tensor", [brow_b], [row_b, pb[bank]], out=row_t[:], in0=ps(bank, rows=1), in1=brow_t[:], op=ALU.add)
        P.dma("sp", mod_d[:, g * 512:(g + 1) * 512], row_t[:], [row_b], [], f"aro{tag}")
        if transpose:
            for j in range(4):
                c = g * 4 + j
                MM(PS[:, 1024 + c:1024 + c + 1], row_t[0:1, j * 128:(j + 1) * 128], ones_f[0:1, 0:1], True, True, [row_b, cbuf], [pb[2]])

    with contextlib.ExitStack() as s0:
        ct = P.sbuf(s0, [128, KC], F32, "ct")
        wt = [P.sbuf(s0, [128, KC, 512], BF16, f"wada{i}") for i in range(3)]
        wtb = bufs(3, "wada")
        row = [P.sbuf(s0, [1, 512], F32, f"row{i}") for i in range(2)]
        rowb = bufs(2, "row")
        brow = [P.sbuf(s0, [1, 512], F32, f"brow{i}") for i in range(2)]
        browb = bufs(2, "brow")
        P.dma("sp", ct[:], I("cT"), [], [b_sil], "s0a")
        ACT(sil[:], ct[:], AF.Silu, [b_sil], [b_sil])
        for g in range(N_S0):
            s = g % 3
            r2 = g % 2
            mod_issue(g, wt[s], wtb[s], brow[r2], browb[r2], f"{s}{r2}")
            mod_compute(g, wt[s], wtb[s], brow[r2], browb[r2], row[r2], rowb[r2], r2, f"{r2}", g < 16)
        b_mod = Buf("modT")
        VOP("dve", "tensor_copy", [], [b_mod, pb[2]], out=modT[:, 0:64], in_=PS[:, 1024:1024 + 64])
        VOP("dve", "tensor_scalar", [b_mod], [b_mod], out=scA1[:], in0=modT[:, 32:64], scalar1=1.0, scalar2=None, op0=ALU.add)
        P.barrier()
    if stop_after <= 0:
        return nc, P

    def proj_group(xsrc, tok0, kside):
        with contextlib.ExitStack() as sg:
            uT = P.sbuf(sg, [128, KC, T], BF16, "uT")
            with contextlib.ExitStack() as s1:
                xt = [P.sbuf(s1, [128, D], F32, f"xt{i}") for i in range(2)]
                xtb = bufs(2, "xt")
                ub = Buf("uT")
                n = 0
                for tt in range(8):
                    s = tt % 2
                    P.dma("sp", xt[s][:], xsrc[tt * 128:(tt + 1) * 128, :], [], [xtb[s]], f"xt{s}")
                    for k4 in range(8):
                        bank = n % 8
                        n += 1
                        for j in range(4):
                            kc = k4 * 4 + j
                            pso = PS[:, bank * 512 + j * 128: bank * 512 + (j + 1) * 128]
                            TR(pso, xt[s][:, kc * 128:(kc + 1) * 128], ident_f[:], [xtb[s], cbuf], [pb[bank]])
                        for j in range(4):
                            kc = k4 * 4 + j
                            pso = PS[:, bank * 512 + j * 128: bank * 512 + (j + 1) * 128]
                            dst = uT[:, kc, tt * 128:(tt + 1) * 128]
                            if bank % 2 == 0:
                                ACT(dst, pso, AF.Identity, [], [pb[bank]], scale=scA1[:, kc:kc + 1], bias=modT[:, kc:kc + 1])
                            else:
                                VOP("dve", "tensor_scalar", [], [pb[bank]], out=dst, in0=pso, scalar1=scA1[:, kc:kc + 1],
                                    scalar2=modT[:, kc:kc + 1], op0=ALU.mult, op1=ALU.add)
                P.barrier()
                if dbg and "uT_d" in dbg and tok0 == 0 and kside:
                    uT_d = dscr("uT_d", [128, KC, T], BF16)
                    P.dma("sp", uT_d, uT[:], [], [], "dbg")
                    P.barrier()
                if stop_after <= 1:
                    return
            with contextlib.ExitStack() as s2:
                wt = [P.sbuf(s2, [128, KC, 512], BF16, f"win{i}") for i in range(2)]
                wtb = bufs(2, "win")
                stg = [P.sbuf(s2, [128, T], F32, f"stg{i}") for i in range(2)]
                stgb = bufs(2, "stg")
                wv = I("w_in").rearrange("(kc p) n -> p kc n", p=128)
                cnt = {"g": 0, "t": 0}

                def fm_group(c0, ncols, epi):
                    s = cnt["g"] % 2
                    cnt["g"] += 1
                    cast_load(wt[s][:, :, 0:ncols], wv[:, :, c0:c0 + ncols], [], [wtb[s]], f"win{s}")
                    for j in range((ncols + 127) // 128):
                        m = min(128, ncols - j * 128)
                        pr = cnt["t"] % 4
                        cnt["t"] += 1
                        for tc in range(2):
                            bank = pr * 2 + tc
                            for kc in range(KC):
                                MM(PS[0:m, bank * 512:(bank + 1) * 512], wt[s][:, kc, j * 128:j * 128 + m],
                                   uT[:, kc, tc * 512:(tc + 1) * 512], kc == 0, kc == KC - 1, [wtb[s]], [pb[bank]])
                        epi(j, PS[0:m, pr * 1024:(pr + 1) * 1024], [pb[pr * 2], pb[pr * 2 + 1]])

                def epi_store(dst_fn, bf):
                    def epi(j, pap, pbs):
                        s = cnt["t"] % 2
                        m = pap.shape[0]
                        if bf:
                            o = stg[s][:].bitcast(BF16)[0:m, 0:T]
                        else:
                            o = stg[s][0:m, :]
                        if cnt["t"] % 2 == 0:
                            ACT(o, pap, AF.Copy, [], [stgb[s]] + pbs)
                        else:
                            VOP("dve", "tensor_copy", [], [stgb[s]] + pbs, out=o, in_=pap)
                        P.dma("sp", dst_fn(j), o, [stgb[s]], [], f"stgo{s}")
                    return epi

                if kside:
                    for g4 in range(4):
                        fm_group(C_KF + g4 * 512, 512, epi_store(lambda j, g4=g4: kT_d[g4 * 4 + j, :, tok0:tok0 + T], True))
                    fm_group(C_CKV, 512, epi_store(lambda j: ckvT_d[j, :, tok0:tok0 + T], False))
                    fm_group(C_KI, 128, epi_store(lambda j: kidxT_d[:, tok0:tok0 + T], False))
                else:
                    for g4 in range(4):
                        fm_group(C_QF + g4 * 512, 512, epi_store(lambda j, g4=g4: qT_d[g4 * 4 + j, :, :], True))
                    for g3 in range(3):
                        fm_group(C_CQ + g3 * 512, 512, epi_store(lambda j, g3=g3: cqT_d[g3 * 4 + j, :, :], False))

                def tm_group(c0, ncols, dst_fn, bf):
                    s = cnt["g"] % 2
                    cnt["g"] += 1
                    cast_load(wt[s][:, :, 0:ncols], wv[:, :, c0:c0 + ncols], [], [wtb[s]], f"win{s}")
                    for tt in range(8):
                        bank = cnt["t"] % 8
                        s2_ = cnt["t"] % 2
                        cnt["t"] += 1
                        for kc in range(KC):
                            MM(PS[:, bank * 512:bank * 512 + ncols], uT[:, kc, tt * 128:(tt + 1) * 128], wt[s][:, kc, 0:ncols],
                               kc == 0, kc == KC - 1, [wtb[s]], [pb[bank]])
                        if bf:
                            o = stg[s2_][:].bitcast(BF16)[:, 0:ncols]
                        else:
                            o = stg[s2_][:, 0:ncols]
                        pap = PS[:, bank * 512:bank * 512 + ncols]
                        if tt % 2 == 0:
                            ACT(o, pap, AF.Copy, [], [stgb[s2_], pb[bank]])
                        else:
                            VOP("dve", "tensor_copy", [], [stgb[s2_], pb[bank]], out=o, in_=pap)
                        P.dma("sp", dst_fn(tt), o, [stgb[s2_]], [], f"stgo{s2_}")

                if kside:
                    for g4 in range(4):
                        tm_group(C_VF + g4 * 512, 512, lambda tt, g4=g4: v_d[tok0 + tt * 128:tok0 + (tt + 1) * 128, g4 * 512:(g4 + 1) * 512], True)
                    tm_group(C_F, 16, lambda tt: flog_d[tok0 + tt * 128:tok0 + (tt + 1) * 128, :], False)
                else:
                    tm_group(C_WI, 32, lambda tt: widx_d[tt * 128:(tt + 1) * 128, :], False)
                P.barrier()

    proj_group(I("x_all")[0:T, :], 0, True)
    if stop_after <= 1:
        return nc, P
    proj_group(I("x_all")[T:S, :], T, True)
    proj_group(I("x_own"), 0, False)
    if stop_after <= 2:
        return nc, P

    def RSQRT(t, B):
        ACT(t, t, AF.Sqrt, [B], [B])
        VOP("dve", "reciprocal", [B], [B], out=t, in_=t)

    def evac(i, out, in_, W):
        if i % 2 == 0:
            return ACT(out, in_, AF.Copy, [], W)
        return VOP("dve", "tensor_copy", [], W, out=out, in_=in_)

    with contextlib.ExitStack() as s3:
        ckvT = P.sbuf(s3, [128, 4, S], F32, "ckvT")
        ckvn = P.sbuf(s3, [128, 4, S], BF16, "ckvn")
        sq = P.sbuf(s3, [128, 4, 512], F32, "sq")
        rstd = P.sbuf(s3, [128, 512], F32, "rstd")
        kvg = P.sbuf(s3, [128, 4], F32, "kvg")
        wuk = P.sbuf(s3, [128, 4, 2048], BF16, "wuk")
        wuv = P.sbuf(s3, [128, 4, 2048], BF16, "wuv")
        stg = [P.sbuf(s3, [128, S], BF16, f"s3stg{i}") for i in range(2)]
        stgb = bufs(2)
        b_ck, b_w, b_n, b_sq, b_rs = Buf(), Buf(), Buf(), Buf(), Buf()
        for r in range(4):
            P.dma("sp", ckvT[:, r, :], ckvT_d[r], [], [b_ck], f"s3l{r % 2}")
        P.dma("sp", kvg[:], I("kvn_g"), [], [b_ck], "s3g")
        cast_load(wuk[:], I("w_uk").rearrange("(r p) n -> p r n", p=128), [], [b_w], "s3w0")
        cast_load(wuv[:], I("w_uv").rearrange("(r p) n -> p r n", p=128), [], [b_w], "s3w1")
        for tc in range(4):
            sl = slice(tc * 512, (tc + 1) * 512)
            bank = tc % 2
            for r in range(4):
                ACT(sq[:, r, :], ckvT[:, r, sl], AF.Square, [b_ck], [b_sq])
            for r in range(4):
                MM(ps(bank), ones_f[:], sq[:, r, :], r == 0, r == 3, [b_sq, cbuf], [pb[bank]])
            VOP("dve", "tensor_scalar", [], [b_rs, pb[bank]], out=rstd[:], in0=ps(bank), scalar1=1.0 / 512, scalar2=1e-6, op0=ALU.mult, op1=ALU.add)
            RSQRT(rstd[:], b_rs)
            for r in range(4):
                VOP("dve", "scalar_tensor_tensor", [b_ck, b_rs], [b_n], out=ckvn[:, r, sl], in0=ckvT[:, r, sl], scalar=kvg[:, r:r + 1],
                    in1=rstd[:], op0=ALU.mult, op1=ALU.mult)
        for h in range(16):
            half = h % 2
            for tc in range(4):
                bank = half * 4 + tc
                for r in range(4):
                    MM(ps(bank), wuk[:, r, h * 128:(h + 1) * 128], ckvn[:, r, tc * 512:(tc + 1) * 512], r == 0, r == 3, [b_w, b_n], [pb[bank]])
            evac(half, stg[half][:], PS[:, half * 2048:(half + 1) * 2048], [stgb[half]] + pb[half * 4:half * 4 + 4])
            P.dma("sp", kdT_d[h], stg[half][:], [stgb[half]], [], f"s3o{half}")
        for st_ in range(16):
            half = st_ % 2
            for cg in range(4):
                bank = half * 4 + cg
                for r in range(4):
                    MM(ps(bank), ckvn[:, r, st_ * 128:(st_ + 1) * 128], wuv[:, r, cg * 512:(cg + 1) * 512], r == 0, r == 3, [b_w, b_n], [pb[bank]])
            evac(half, stg[half][:], PS[:, half * 2048:(half + 1) * 2048], [stgb[half]] + pb[half * 4:half * 4 + 4])
            P.dma("sp", vd_d[st_ * 128:(st_ + 1) * 128, :], stg[half][:], [stgb[half]], [], f"s3o{half}")
        P.barrier()
    with contextlib.ExitStack() as s3:
        kx = P.sbuf(s3, [128, S], F32, "kx")
        kg = P.sbuf(s3, [128, 2], F32, "kg")
        sqk = P.sbuf(s3, [128, 512], F32, "sqk")
        mean = P.sbuf(s3, [128, 512], F32, "mean")
        var = P.sbuf(s3, [128, 512], F32, "var")
        xm = P.sbuf(s3, [128, 512], F32, "xm")
        b_kx, b_t = Buf(), Buf()
        P.dma("sp", kx[:], kidxT_d, [], [b_kx], "s3k")
        P.dma("sp", kg[:], I("ki_gb"), [], [b_kx], "s3kg")
        for tc in range(4):
            sl = slice(tc * 512, (tc + 1) * 512)
            ACT(sqk[:], kx[:, sl], AF.Square, [b_kx], [b_t])
            MM(ps(0), ones_f[:], kx[:, sl], True, True, [b_kx, cbuf], [pb[0]])
            MM(ps(1), ones_f[:], sqk[:], True, True, [b_t, cbuf], [pb[1]])
            VOP("dve", "tensor_scalar", [], [b_t, pb[0]], out=mean[:], in0=ps(0), scalar1=1.0 / 128, scalar2=None, op0=ALU.mult)
            VOP("dve", "tensor_scalar", [], [b_t, pb[1]], out=var[:], in0=ps(1), scalar1=1.0 / 128, scalar2=None, op0=ALU.mult)
            VOP("dve", "tensor_tensor", [b_t], [b_t], out=xm[:], in0=mean[:], in1=mean[:], op=ALU.mult)
            VOP("dve", "tensor_tensor", [b_t], [b_t], out=var[:], in0=var[:], in1=xm[:], op=ALU.subtract)
            VOP("dve", "tensor_scalar", [b_t], [b_t], out=var[:], in0=var[:], scalar1=1e-5, scalar2=None, op0=ALU.add)
            RSQRT(var[:], b_t)
            VOP("dve", "tensor_tensor", [b_t, b_kx], [b_t], out=xm[:], in0=kx[:, sl], in1=mean[:], op=ALU.subtract)
            VOP("dve", "tensor_tensor", [b_t], [b_t], out=xm[:], in0=xm[:], in1=var[:], op=ALU.mult)
            VOP("dve", "tensor_scalar", [b_t], [cbuf], out=kiT[:, sl], in0=xm[:], scalar1=kg[:, 0:1], scalar2=kg[:, 1:2], op0=ALU.mult, op1=ALU.add)
        fl = P.sbuf(s3, [128, 16, 16], F32, "fl")
        bfg = P.sbuf(s3, [128, 16], F32, "bfg")
        lf80 = P.sbuf(s3, [128, 16, 80], F32, "lf80")
        tri_i = P.sbuf(s3, [128, 128], F32, "tri_i")
        b_f = Buf()
        P.dma("sp", fl[:], flog_d.rearrange("(n p) h -> p n h", p=128), [], [b_f], "s3f")
        P.dma("sp", bfg[:], I("b_forget").partition_broadcast(128), [], [b_f], "s3fb")
        P.dma("sp", tri_i[:], I("triu"), [], [b_f], "s3ft")
        VOP("dve", "tensor_tensor", [b_f, cbuf], [b_f], out=tri_i[:], in0=tri_i[:], in1=ident_f[:], op=ALU.add)
        for n_ in range(16):
            VOP("dve", "tensor_tensor", [b_f], [b_f], out=fl[:, n_, :], in0=fl[:, n_, :], in1=bfg[:], op=ALU.add)
        ACT(fl[:], fl[:], AF.Exp, [b_f], [b_f], scale=-1.0)
        ACT(fl[:], fl[:], AF.Ln, [b_f], [b_f], bias=1.0)
        VOP("dve", "memset", [], [b_f], lf80[:], 0.0)
        for o_ in (0, 32, 64):
            VOP("dve", "tensor_scalar", [b_f], [b_f], out=lf80[:, :, o_:o_ + 16], in0=fl[:], scalar1=-1.0, scalar2=None, op0=ALU.mult)
        for j in range(16):
            out = PS[0:80, j * 128:(j + 1) * 128]
            for jp in range(j):
                MM(out, lf80[:, jp, :], ones_f[:], jp == 0, False, [b_f, cbuf], [pb[j // 4]])
            MM(out, lf80[:, j, :], tri_i[:], j == 0, True, [b_f], [pb[j // 4]])
        v32 = P.sbuf(s3, [80, S], F32, "v32")
        hf = P.sbuf(s3, [80, S], F32, "hf")
        hb = P.sbuf(s3, [80, S], BF16, "hb")
        b_c = Buf()
        VOP("dve", "tensor_scalar", [], [b_c] + pb[0:4], out=v32[:], in0=PS[0:80, 0:2048], scalar1=-1.0 / SCALE, scalar2=None, op0=ALU.mult)
        VOP("dve", "memset", [], [cbuf], CS[:], 0.0)
        for k_, o_ in enumerate((0, 32, 64)):
            VOP("dve", "tensor_copy", [b_c], [b_c], out=hb[:], in_=v32[:])
            VOP("dve", "tensor_copy", [b_c], [cbuf], out=CS[o_:o_ + 16, :], in_=hb[o_:o_ + 16, :])
            if k_ < 2:
                VOP("dve", "tensor_copy", [b_c], [b_c], out=hf[:], in_=hb[:])
                VOP("dve", "tensor_tensor", [b_c], [b_c], out=v32[:], in0=v32[:], in1=hf[:], op=ALU.subtract)
        P.barrier()
    if dbg and "cs_dbg" in dbg:
        dd = dscr("cs_dbg", [80, S], BF16)
        P.dma("sp", dd, CS[:], [], [], "dbg")
        dd2 = dscr("ki_dbg", [128, S], BF16)
        P.dma("sp", dd2, kiT[:], [], [], "dbg2")
        P.barrier()
    if stop_after <= 3:
        return nc, P

    with contextlib.ExitStack() as s5:
        cq = P.sbuf(s5, [128, 12, T], F32, "cq")
        cqn = P.sbuf(s5, [128, 12, T], BF16, "cqn")
        sq = P.sbuf(s5, [128, 12, 512], F32, "sq5")
        rstd = P.sbuf(s5, [128, 512], F32, "rstd5")
        qg = P.sbuf(s5, [128, 12], F32, "qg")
        wt = [P.sbuf(s5, [128, 12, 512], BF16, f"w5_{i}") for i in range(2)]
        wtb = bufs(2)
        stg = [P.sbuf(s5, [128, T], BF16, f"s5stg{i}") for i in range(2)]
        stgb = bufs(2)
        b_cq, b_sq, b_rs, b_n = Buf(), Buf(), Buf(), Buf()
        for r in range(12):
            P.dma("sp", cq[:, r, :], cqT_d[r], [], [b_cq], f"s5l{r % 2}")
        P.dma("sp", qg[:], I("qn_g"), [], [b_cq], "s5g")
        for tc in range(2):
            sl = slice(tc * 512, (tc + 1) * 512)
            for r in range(12):
                ACT(sq[:, r, :], cq[:, r, sl], AF.Square, [b_cq], [b_sq])
            for r in range(12):
                MM(ps(tc), ones_f[:], sq[:, r, :], r == 0, r == 11, [b_sq, cbuf], [pb[tc]])
            VOP("dve", "tensor_scalar", [], [b_rs, pb[tc]], out=rstd[:], in0=ps(tc), scalar1=1.0 / 1536, scalar2=1e-6, op0=ALU.mult, op1=ALU.add)
            RSQRT(rstd[:], b_rs)
            for r in range(12):
                VOP("dve", "scalar_tensor_tensor", [b_cq, b_rs], [b_n], out=cqn[:, r, sl], in0=cq[:, r, sl], scalar=qg[:, r:r + 1],
                    in1=rstd[:], op0=ALU.mult, op1=ALU.mult)
        n = 0
        for g in range(12):
            s = g % 2
            if g < 4:
                wsrc = I("w_uq").rearrange("(r p) n -> p r n", p=128)[:, :, g * 512:(g + 1) * 512]
            else:
                wsrc = I("w_iq").rearrange("(r p) n -> p r n", p=128)[:, :, (g - 4) * 512:(g - 3) * 512]
            cast_load(wt[s][:], wsrc, [], [wtb[s]], f"s5w{s}")
            for j in range(4):
                pr = n % 4
                n += 1
                for tc in range(2):
                    bank = pr * 2 + tc
                    for r in range(12):
                        MM(ps(bank), wt[s][:, r, j * 128:(j + 1) * 128], cqn[:, r, tc * 512:(tc + 1) * 512], r == 0, r == 11, [wtb[s], b_n], [pb[bank]])
                s2_ = n % 2
                evac(n, stg[s2_][:], PS[:, pr * 1024:(pr + 1) * 1024], [stgb[s2_], pb[pr * 2], pb[pr * 2 + 1]])
                dst = qdT_d[g * 4 + j] if g < 4 else qiT_d[(g - 4) * 4 + j]
                P.dma("sp", dst, stg[s2_][:], [stgb[s2_]], [], f"s5o{s2_}")
        P.barrier()
    if stop_after <= 5:
        return nc, P

    NIT = 22
    with contextlib.ExitStack() as s7:
        qi_t = [P.sbuf(s7, [128, 32, 128], BF16, f"qi{i}") for i in range(2)]
        qib = bufs(2)
        wi = P.sbuf(s7, [128, NQT, 32], F32, "wi")
        absw = P.sbuf(s7, [128, NQT, 32], F32, "absw")
        sgn = P.sbuf(s7, [128, NQT, 32], F32, "sgn")
        acc = [P.sbuf(s7, [128, S], F32, f"acc{i}") for i in range(1)]
        accb = bufs(2)
        rr = [P.sbuf(s7, [128, S], F32, f"rr{i}") for i in range(2)]
        rrb = bufs(2)
        junk = P.sbuf(s7, [128, S], BF16, "junk")
        selm = [P.sbuf(s7, [128, S], BF16, f"selm{i}") for i in range(2)]
        selb = bufs(2)
        sm = P.sbuf(s7, [128, 8], F32, "sm")
        b_w, b_s = Buf(), Buf()
        P.dma("sp", wi[:], widx_d.rearrange("(i p) h -> p i h", p=128), [], [b_w], "s7w")
        VOP("dve", "tensor_scalar", [b_w], [b_w], out=sgn[:], in0=wi[:], scalar1=-1.0, scalar2=None, op0=ALU.mult)
        VOP("dve", "tensor_tensor", [b_w], [b_w], out=absw[:], in0=wi[:], in1=sgn[:], op=ALU.max)
        VOP("dve", "tensor_scalar", [b_w], [b_w], out=absw[:], in0=absw[:], scalar1=1.0 / 64, scalar2=None, op0=ALU.mult)
        VOP("dve", "tensor_scalar", [b_w], [b_w], out=sgn[:], in0=wi[:], scalar1=0.0, scalar2=None, op0=ALU.is_ge)
        VOP("dve", "tensor_scalar", [b_w], [b_w], out=sgn[:], in0=sgn[:], scalar1=2.0, scalar2=-1.0, op0=ALU.mult, op1=ALU.add)
        VOP("dve", "memset", [], [b_s], sm[:, 6:7], 0.5)
        qsrc = qiT_d.rearrange("h p t -> p h t")
        mwt = [P.sbuf(s7, [128, KC, 512], BF16, f"mwt{i}") for i in range(3)]
        mwtb = bufs(3)
        mrow = [P.sbuf(s7, [1, 512], F32, f"mrow{i}") for i in range(3)]
        mrowb = bufs(3)
        mbrow = [P.sbuf(s7, [1, 512], F32, f"mbrow{i}") for i in range(3)]
        mbrowb = bufs(3)

        def mod_bg_compute(i_prev):
            for j in range(3):
                g = N_S0 + i_prev * 3 + j
                mod_compute(g, mwt[j], mwtb[j], mbrow[j], mbrowb[j], mrow[j], mrowb[j], 0, f"m{j}", False)

        for i in range(NQT):
            s = i % 2
            nk = 2 * i + 2
            ncol = nk * 128
            nch = (ncol + 511) // 512
            if i > 0:
                mod_bg_compute(i - 1)
            for j in range(3):
                g = N_S0 + i * 3 + j
                mod_issue(g, mwt[j], mwtb[j], mbrow[j], mbrowb[j], f"m{j}")
            P.dma("sp", qi_t[s][:], qsrc[:, :, i * 128:(i + 1) * 128], [], [qib[s]], f"s7q{s}")
            VOP("dve", "memset", [], [accb[0]], acc[0][:, 0:ncol], 0.0)
            VOP("dve", "tensor_copy", [cbuf], [accb[0]], out=acc[0][:, ncol - 256:ncol], in_=maskbig[:])
            for hi_ in range(32):
                sl = hi_ % 2
                for c in range(nch):
                    cw = min(512, ncol - c * 512)
                    bank = sl * 4 + c
                    MM(PS[:, bank * 512:bank * 512 + cw], qi_t[s][:, hi_, :], kiT[:, c * 512:c * 512 + cw], True, True, [qib[s], cbuf], [pb[bank]])
                ACT(rr[sl][:, 0:ncol], PS[:, sl * 2048:sl * 2048 + ncol], AF.Relu, [b_w], [rrb[sl]] + pb[sl * 4:sl * 4 + nch],
                    scale=absw[:, i, hi_:hi_ + 1])
                VOP("dve", "scalar_tensor_tensor", [rrb[sl], b_w], [accb[0]], out=acc[0][:, 0:ncol], in0=rr[sl][:, 0:ncol],
                    scalar=sgn[:, i, hi_:hi_ + 1], in1=acc[0][:, 0:ncol], op0=ALU.mult, op1=ALU.add)
            A = acc[0][:, 0:ncol]
            VOP("dve", "tensor_reduce", [accb[0]], [b_s], out=sm[:, 1:2], in_=A, axis=AX.X, op=ALU.max)
            VOP("dve", "tensor_scalar", [b_s], [b_s], out=sm[:, 0:1], in0=sm[:, 1:2], scalar1=-64.0, scalar2=None, op0=ALU.add)
            for it in range(NIT):
                hw_ = 32.0 / (2 ** it)
                VOP("dve", "tensor_scalar", [b_s], [b_s], out=sm[:, 2:3], in0=sm[:, 0:1], scalar1=hw_, scalar2=None, op0=ALU.add)
                VOP("dve", "memset", [], [b_s], sm[:, 3:4], 0.0)
                VOP("dve", "tensor_scalar", [accb[0], b_s], [b_s], out=junk[:, 0:ncol], in0=A, scalar1=sm[:, 2:3], scalar2=0.0, op0=ALU.is_ge, op1=ALU.add,
                    accum_out=sm[:, 3:4])
                VOP("dve", "tensor_scalar", [b_s], [b_s], out=sm[:, 4:5], in0=sm[:, 3:4], scalar1=255.5, scalar2=hw_, op0=ALU.is_ge, op1=ALU.mult)
                VOP("dve", "tensor_tensor", [b_s], [b_s], out=sm[:, 0:1], in0=sm[:, 0:1], in1=sm[:, 4:5], op=ALU.add)
            VOP("dve", "tensor_scalar", [accb[0], b_s], [selb[s]], out=selm[s][:, 0:ncol], in0=A, scalar1=sm[:, 0:1], scalar2=NEGBIG, op0=ALU.is_lt, op1=ALU.mult)
            P.dma("sp", sel_d[i][:, 0:ncol], selm[s][:, 0:ncol], [selb[s]], [], f"s7o{s}")
        mod_bg_compute(NQT - 1)
        P.barrier()
    if stop_after <= 7:
        return nc, P

    mT_d = dscr("mT_d", [KC, 128, T], BF16)
    sA = contextlib.ExitStack()
    attn = P.sbuf(sA, [128, NQT, D], BF16, "attn")
    ab = Buf("attn")
    with contextlib.ExitStack() as s8:
        kT_h = [P.sbuf(s8, [128, S], BF16, f"kTh{i}") for i in range(2)]
        qT_h = [P.sbuf(s8, [128, T], BF16, f"qTh{i}") for i in range(2)]
        v_h = [P.sbuf(s8, [128, 16, 132], BF16, f"vh{i}") for i in range(2)]
        G_h = [P.sbuf(s8, [128, 2304], BF16, f"Gh{i}") for i in range(2)]
        hb_ = bufs(2)
        Gf = [P.sbuf(s8, [128, 2304], F32, f"Gf{i}") for i in range(2)]
        gfb = bufs(2)
        selall = P.sbuf(s8, [128, NQT, S], BF16, "selall")
        b_sel = Buf()
        Pt = [P.sbuf(s8, [128, S], BF16, f"Pt{i}") for i in range(2)]
        ptb = bufs(2)
        PT = [P.sbuf(s8, [128, S], BF16, f"PT{i}") for i in range(2)]
        pTb = bufs(2)
        sm = P.sbuf(s8, [128, 8], F32, "sm8")
        smb = bufs(2)
        for s in range(2):
            VOP("dve", "memset", [], [hb_[s]], v_h[s][:, :, 128:132], 1.0)
        VOP("dve", "memset", [], [b_sel], selall[:], 0.0)
        for i in range(NQT):
            ncol = (2 * i + 2) * 128
            P.dma("sp", selall[:, i, 0:ncol], sel_d[i][:, 0:ncol], [], [b_sel], f"s8s{i % 2}")
        def head_loads(hd):
            fox = hd < 16
            h = hd % 16
            s = hd % 2
            P.dma("sp", kT_h[s][:], (kT_d if fox else kdT_d)[h], [], [hb_[s]], f"s8k{s}")
            P.dma("sp", qT_h[s][:], (qT_d if fox else qdT_d)[h], [], [hb_[s]], f"s8q{s}")
            vsrc = (v_d if fox else vd_d).rearrange("(n p) c -> p n c", p=128)[:, :, h * 128:(h + 1) * 128]
            P.dma("sp", v_h[s][:, :, 0:128], vsrc, [], [hb_[s]], f"s8v{s}")
            if not fox:
                P.dma("sp", Gf[s][:], I("biasG")[h], [], [gfb[s]], f"s8g{s}")
                VOP("dve", "tensor_scalar", [gfb[s]], [hb_[s]], out=G_h[s][:], in0=Gf[s][:], scalar1=1.0 / SCALE, scalar2=None, op0=ALU.mult)

        def stage_A(n):
            hd, i = divmod(n, NQT)
            fox = hd < 16
            h = hd % 16
            s = hd % 2
            s2 = n % 2
            nk = 2 * i + 2
            ncol = nk * 128
            nch = (ncol + 511) // 512
            qsl = qT_h[s][:, i * 128:(i + 1) * 128]
            for c in range(nch):
                cw = min(512, ncol - c * 512)
                out = PS[:, c * 512:c * 512 + cw]
                ksl = slice(c * 512, c * 512 + cw)
                MM(out, qsl, kT_h[s][:, ksl], True, False, [hb_[s]], [pb[c]])
                if fox:
                    if c == nch - 1:
                        MM(PS[:, ncol - 256:ncol], ident_b[:], maskb[:], False, False, [cbuf], [pb[c]])
                    MM(out, Esel[0:80, h * 128:(h + 1) * 128], CS[0:80, ksl], False, True, [cbuf], [pb[c]])
                else:
                    n0 = 2048 - 256 * i + c * 512
                    MM(out, ident_b[:], G_h[s][:, n0:n0 + cw], False, False, [cbuf, hb_[s]], [pb[c]])
                    MM(out, ident_b[:], selall[:, i, ksl], False, True, [cbuf, b_sel], [pb[c]])
            sc_ps = PS[:, 0:ncol]
            VOP("dve", "tensor_reduce", [], [smb[s2]] + pb[0:nch], out=sm[:, s2:s2 + 1], in_=sc_ps, axis=AX.X, op=ALU.max)
            VOP("dve", "tensor_scalar", [smb[s2]], [smb[s2]], out=sm[:, 2 + s2:3 + s2], in0=sm[:, s2:s2 + 1], scalar1=-SCALE, scalar2=None, op0=ALU.mult)
            ACT(Pt[s2][:, 0:ncol], sc_ps, AF.Exp, [smb[s2]], [ptb[s2]] + pb[0:nch], scale=SCALE, bias=sm[:, 2 + s2:3 + s2])

        def stage_B(n):
            hd, i = divmod(n, NQT)
            s2 = n % 2
            nk = 2 * i + 2
            ncol = nk * 128
            for kb in range(nk):
                TR(PSB[:, 4096 + kb * 128:4096 + (kb + 1) * 128], Pt[s2][:, kb * 128:(kb + 1) * 128], ident_b[:], [ptb[s2], cbuf], [pb[4 + kb // 8]])
            nb2 = (nk + 7) // 8
            evac(n, PT[s2][:, 0:ncol], PSB[:, 4096:4096 + ncol], [pTb[s2]] + pb[4:4 + nb2])

        def stage_C(n):
            hd, i = divmod(n, NQT)
            fox = hd < 16
            h = hd % 16
            s = hd % 2
            s2 = n % 2
            nk = 2 * i + 2
            ob = 6 + s2
            for kb in range(nk):
                MM(PS[:, ob * 512:ob * 512 + 129], PT[s2][:, kb * 128:(kb + 1) * 128], v_h[s][:, kb, 0:129], kb == 0, kb == nk - 1, [pTb[s2], hb_[s]], [pb[ob]])
            VOP("dve", "reciprocal", [], [smb2[s2], pb[ob]], out=sm[:, 4 + s2:5 + s2], in_=PS[:, ob * 512 + 128:ob * 512 + 129])
            col0 = (0 if fox else 2048) + h * 128
            ACT(attn[:, i, col0:col0 + 128], PS[:, ob * 512:ob * 512 + 128], AF.Copy, [smb2[s2]], [ab, pb[ob]], scale=sm[:, 4 + s2:5 + s2])

        smb2 = bufs(2)
        NIT8 = 32 * NQT
        for n in range(NIT8):
            if n % NQT == 0:
                head_loads(n // NQT)
            stage_A(n)
            if n > 0:
                stage_C(n - 1)
            stage_B(n)
        stage_C(NIT8 - 1)
        P.barrier()
    if dbg and "attn_d" in dbg:
        for i in range(NQT):
            P.dma("sp", attn_d[i * 128:(i + 1) * 128, :], attn[:, i, :], [], [], "dbg")
        P.barrier()
    if stop_after <= 8:
        return nc, P

    with contextlib.ExitStack() as s9:
        tmp = P.sbuf(s9, [128, 2048], F32, "tmp9")
        ssq = P.sbuf(s9, [128, 16], F32, "ssq")
        mg = P.sbuf(s9, [128, KC], F32, "mg")
        b_t, b_q = Buf(), Buf()
        P.dma("sp", mg[:], I("mg_g"), [], [b_q], "s9g")
        for i in range(NQT):
            for grp in range(2):
                seg = attn[:, i, grp * 2048:(grp + 1) * 2048]
                VOP("dve", "tensor_tensor", [ab], [b_t], out=tmp[:], in0=seg, in1=seg, op=ALU.mult)
                VOP("dve", "tensor_reduce", [b_t], [b_q], out=ssq[:, i * 2 + grp:i * 2 + grp + 1], in_=tmp[:], axis=AX.X, op=ALU.add)
        VOP("dve", "tensor_scalar", [b_q], [b_q], out=ssq[:], in0=ssq[:], scalar1=1.0 / 2048, scalar2=1e-6, op0=ALU.mult, op1=ALU.add)
        RSQRT(ssq[:], b_q)
        for i in range(NQT):
            for grp in range(2):
                seg = attn[:, i, grp * 2048:(grp + 1) * 2048]
                VOP("dve", "tensor_scalar", [b_q], [ab], out=seg, in0=seg, scalar1=ssq[:, i * 2 + grp:i * 2 + grp + 1], scalar2=None, op0=ALU.mult)
        n = 0
        mst = [P.sbuf(s9, [128, 8, 128], BF16, f"mst{i}") for i in range(4)]
        mstb = bufs(4)
        mTv = mT_d.rearrange("k p t -> p k t")
        for i in range(NQT):
            for k8 in range(4):
                bank = 4 + (n % 4)
                ms = n % 4
                n += 1
                for j in range(8):
                    kc = k8 * 8 + j
                    TR(PSB[:, bank * 1024 + j * 128:bank * 1024 + (j + 1) * 128], attn[:, i, kc * 128:(kc + 1) * 128], ident_b[:], [ab, cbuf], [pb[bank]])
                for j in range(8):
                    kc = k8 * 8 + j
                    src_ = PSB[:, bank * 1024 + j * 128:bank * 1024 + (j + 1) * 128]
                    dst = mst[ms][:, j, :]
                    if bank % 2 == 0:
                        ACT(dst, src_, AF.Copy, [b_q], [mstb[ms], pb[bank]], scale=mg[:, kc:kc + 1])
                    else:
                        VOP("dve", "tensor_scalar", [b_q], [mstb[ms], pb[bank]], out=dst, in0=src_, scalar1=mg[:, kc:kc + 1], scalar2=None, op0=ALU.mult)
                P.dma("sp", mTv[:, k8 * 8:(k8 + 1) * 8, i * 128:(i + 1) * 128], mst[ms][:], [mstb[ms]], [], f"s9m{ms}")
        P.barrier()
    sA.close()

    with contextlib.ExitStack() as s9:
        wt = [P.sbuf(s9, [128, KC, 512], BF16, f"wo{i}") for i in range(2)]
        wtb = bufs(2)
        GA = P.sbuf(s9, [128, D], F32, "GA")
        b_ga = Buf()
        xt = [P.sbuf(s9, [128, 512], F32, f"x9_{i}") for i in range(3)]
        xtb = bufs(3)
        t1 = [P.sbuf(s9, [128, 512], F32, f"t9_{i}") for i in range(2)]
        t1b = bufs(2)
        rt = [P.sbuf(s9, [128, 512], F32, f"r9_{i}") for i in range(2)]
        rtb = bufs(2)
        P.dma("sp", GA[:], mod_d[:, 2 * D:3 * D].partition_broadcast(128), [], [b_ga], "s9ga")
        mT = P.sbuf(s9, [128, KC, T], BF16, "mT")
        P.dma("sp", mT[:], mT_d.rearrange("k p t -> p k t"), [], [b_ga], "s9mt")
        wv = I("w_o").rearrange("(kc p) n -> p kc n", p=128)
        n = 0
        for dg in range(8):
            s = dg % 2
            dsl = slice(dg * 512, (dg + 1) * 512)
            cast_load(wt[s][:], wv[:, :, dsl], [], [wtb[s]], f"s9w{s}")
            for i in range(NQT):
                bank = n % 8
                s3_ = n % 3
                s2 = n % 2
                n += 1
                P.dma("sp", xt[s3_][:], I("x_own")[i * 128:(i + 1) * 128, dsl], [], [xtb[s3_]], f"s9x{s3_}")
                for kc in range(KC):
                    MM(ps(bank), mT[:, kc, i * 128:(i + 1) * 128], wt[s][:, kc, :], kc == 0, kc == KC - 1, [wtb[s], b_ga], [pb[bank]])
                VOP("dve", "tensor_tensor", [b_ga], [t1b[s2], pb[bank]], out=t1[s2][:], in0=ps(bank), in1=GA[:, dsl], op=ALU.mult)
                VOP("dve", "scalar_tensor_tensor", [xtb[s3_], t1b[s2]], [rtb[s2]], out=rt[s2][:], in0=xt[s3_][:], scalar=ALPHA, in1=t1[s2][:], op0=ALU.mult, op1=ALU.add)
                P.dma("sp", r_d[i * 128:(i + 1) * 128, dsl], rt[s2][:], [rtb[s2]], [], f"s9o{s2}")
        P.barrier()
    if stop_after <= 9:
        return nc, P

    sR = contextlib.ExitStack()
    gi = P.sbuf(sR, [128, 64], I32, "gi")
    sg = P.sbuf(sR, [128, 64], F32, "sg")
    rki = P.sbuf(sR, [128, NQT * 4], I32, "rki")
    b_rt = Buf("routing")
    sL = contextlib.ExitStack()
    LGT = P.sbuf(sL, [128, NQT, NE], F32, "LGT")
    with contextlib.ExitStack() as s10:
        LG = P.sbuf(s10, [128, D], F32, "LG")
        LB = P.sbuf(s10, [128, D], F32, "LB")
        A2 = P.sbuf(s10, [128, D], F32, "A2")
        B2 = P.sbuf(s10, [128, D], F32, "B2")
        rt = [P.sbuf(s10, [128, D], F32, f"rt{i}") for i in range(2)]
        rtb = bufs(2)
        tmp = P.sbuf(s10, [128, D], F32, "tmp10")
        u2f = P.sbuf(s10, [128, D], F32, "u2f")
        x1t = P.sbuf(s10, [128, D], F32, "x1t")
        u2b = P.sbuf(s10, [128, D], BF16, "u2b")
        u2T = P.sbuf(s10, [128, KC, 128], F32, "u2T")
        wr = P.sbuf(s10, [128, KC, NE], F32, "wr")
        brt = P.sbuf(s10, [128, NE], F32, "brt")
        st = P.sbuf(s10, [128, 4], F32, "st10")
        b_c, b_tmp, b_u, b_x1, b_ub, b_uT, b_st, b_lg = Buf(), Buf(), Buf(), Buf(), Buf(), Buf(), Buf(), Buf()
        P.dma("sp", LG[:], I("ln1_g").partition_broadcast(128), [], [b_c], "s10a")
        P.dma("sp", LB[:], I("ln1_b").partition_broadcast(128), [], [b_c], "s10b")
        P.dma("sp", tmp[:], mod_d[:, 4 * D:5 * D].partition_broadcast(128), [], [b_c], "s10a")
        P.dma("sp", u2f[:], mod_d[:, 3 * D:4 * D].partition_broadcast(128), [], [b_c], "s10b")
        P.dma("sp", wr[:], I("w_router").rearrange("(kc p) e -> p kc e", p=128), [], [b_c], "s10a")
        P.dma("sp", brt[:], I("b_router").partition_broadcast(128), [], [b_c], "s10b")
        VOP("dve", "scalar_tensor_tensor", [b_c], [b_c], out=A2[:], in0=tmp[:], scalar=1.0, in1=LG[:], op0=ALU.add, op1=ALU.mult)
        VOP("dve", "scalar_tensor_tensor", [b_c], [b_c], out=B2[:], in0=tmp[:], scalar=1.0, in1=LB[:], op0=ALU.add, op1=ALU.mult)
        VOP("dve", "tensor_tensor", [b_c], [b_c], out=B2[:], in0=B2[:], in1=u2f[:], op=ALU.add)
        P.barrier()
        for i in range(NQT):
            s = i % 2
            R = rt[s]
            rsl = slice(i * 128, (i + 1) * 128)
            P.dma("sp", R[:], r_d[rsl, :], [], [rtb[s]], f"s10r{s}")
            VOP("dve", "tensor_reduce", [rtb[s]], [b_st], out=st[:, 0:1], in_=R[:], axis=AX.X, op=ALU.add)
            VOP("dve", "tensor_scalar", [b_st], [b_st], out=st[:, 1:2], in0=st[:, 0:1], scalar1=-1.0 / D, scalar2=None, op0=ALU.mult)
            ACT(tmp[:], R[:], AF.Square, [rtb[s], b_st], [b_tmp], bias=st[:, 1:2])
            VOP("dve", "tensor_reduce", [b_tmp], [b_st], out=st[:, 2:3], in_=tmp[:], axis=AX.X, op=ALU.add)
            VOP("dve", "tensor_scalar", [b_st], [b_st], out=st[:, 2:3], in0=st[:, 2:3], scalar1=1.0 / D, scalar2=1e-5, op0=ALU.mult, op1=ALU.add)
            RSQRT(st[:, 2:3], b_st)
            VOP("dve", "tensor_scalar", [b_st], [rtb[s]], out=R[:], in0=R[:], scalar1=st[:, 1:2], scalar2=st[:, 2:3], op0=ALU.add, op1=ALU.mult)
            VOP("pool", "tensor_tensor", [rtb[s], b_c], [b_x1], out=x1t[:], in0=R[:], in1=LG[:], op=ALU.mult)
            VOP("pool", "tensor_tensor", [b_c], [b_x1], out=x1t[:], in0=x1t[:], in1=LB[:], op=ALU.add)
            P.dma("sp", x1_d[rsl, :], x1t[:], [b_x1], [], "s10x")
            VOP("dve", "tensor_tensor", [rtb[s], b_c], [b_u], out=u2f[:], in0=R[:], in1=A2[:], op=ALU.mult)
            VOP("dve", "tensor_tensor", [b_c], [b_u], out=u2f[:], in0=u2f[:], in1=B2[:], op=ALU.add)
            ACT(u2b[:], u2f[:], AF.Copy, [b_u], [b_ub])
            P.dma("sp", u2_d[rsl, :], u2b[:], [b_ub], [], "s10u")
            for k4 in range(8):
                bank = k4 % 4
                for j in range(4):
                    kc = k4 * 4 + j
                    TR(PS[:, bank * 512 + j * 128:bank * 512 + (j + 1) * 128], u2f[:, kc * 128:(kc + 1) * 128], ident_f[:], [b_u, cbuf], [pb[bank]])
                evac(bank, u2T[:, k4 * 4:(k4 + 1) * 4, :], PS[:, bank * 512:(bank + 1) * 512], [b_uT, pb[bank]])
            lb_ = 4 + (i % 2)
            for kc in range(KC):
                MM(PS[:, lb_ * 512:lb_ * 512 + NE], u2T[:, kc, :], wr[:, kc, :], kc == 0, kc == KC - 1, [b_uT, b_c], [pb[lb_]])
            VOP("dve", "tensor_tensor", [b_c], [b_lg, pb[lb_]], out=LGT[:, i, :], in0=PS[:, lb_ * 512:lb_ * 512 + NE], in1=brt[:], op=ALU.add)
        P.barrier()
    with contextlib.ExitStack() as sb:
        mx8 = P.sbuf(sb, [128, NQT, 8], F32, "mx8")
        idx8 = P.sbuf(sb, [128, NQT, 8], U32, "idx8")
        idxf = P.sbuf(sb, [128, NQT, 8], F32, "idxf")
        MK = P.sbuf(sb, [128, NQT, NE], F32, "MK")
        GT = P.sbuf(sb, [128, NQT, NE], F32, "GT")
        MKb = P.sbuf(sb, [128, NQT, NE], BF16, "MKb")
        POS = P.sbuf(sb, [128, NQT, NE], F32, "POS")
        RF = P.sbuf(sb, [128, NQT, NE], F32, "RF")
        VL = P.sbuf(sb, [128, NQT, NE], F32, "VL")
        iot = P.sbuf(sb, [128, 256], F32, "iot")
        tix = P.sbuf(sb, [128, 16], F32, "tix")
        trs = P.sbuf(sb, [128, 128], F32, "trs")
        trsb = P.sbuf(sb, [128, 128], BF16, "trsb")
        ecol = P.sbuf(sb, [128, NE], F32, "ecol")
        s1 = P.sbuf(sb, [128, 16], F32, "s1")
        oh = P.sbuf(sb, [128, NE], F32, "oh")
        rkf = P.sbuf(sb, [128, NQT * 4], F32, "rkf")
        Se = [P.sbuf(sb, [128, NQT, CAP], BF16, f"Se{i}") for i in range(2)]
        R5 = P.sbuf(sb, [128, NQT, NE, 5], BF16, "R5")
        gtmp = P.sbuf(sb, [128, NQT, NE], F32, "gtmp")
        gres = P.sbuf(sb, [128, NQT, NE], F32, "gres")
        Seb = bufs(2)
        SI = P.sbuf(sb, [128, 64, 5], F32, "SI")
        vtmp = P.sbuf(sb, [128, 64], F32, "vtmp")
        gf = P.sbuf(sb, [128, 64], F32, "gf")
        b_k = Buf()
        P.dma("sp", iot[:], I("iota"), [], [b_k], "s10a")
        P.dma("sp", tix[:], I("tidx"), [], [b_k], "s10b")
        P.dma("sp", trs[:], I("triu"), [], [b_k], "s10a")
        VOP("dve", "tensor_copy", [b_k], [b_k], out=trsb[:], in_=trs[:])
        VOP("dve", "tensor_scalar", [b_k], [b_k], out=ecol[:], in0=iot[:, 0:NE], scalar1=float(CAP), scalar2=None, op0=ALU.mult)
        for i in range(NQT):
            VOP("dve", "max", [b_lg], [b_k], out=mx8[:, i, :], in_=LGT[:, i, :])
            VOP("dve", "max_index", [b_lg, b_k], [b_k], out=idx8[:, i, :], in_max=mx8[:, i, :], in_values=LGT[:, i, :])
            VOP("dve", "tensor_scalar", [b_lg, b_k], [b_k], out=MK[:, i, :], in0=LGT[:, i, :], scalar1=mx8[:, i, 3:4], scalar2=None, op0=ALU.is_ge)
            VOP("dve", "tensor_scalar", [b_k], [b_k], out=s1[:, i:i + 1], in0=mx8[:, i, 0:1], scalar1=-1.0, scalar2=None, op0=ALU.mult)
            ACT(GT[:, i, :], LGT[:, i, :], AF.Exp, [b_lg, b_k], [b_k], bias=s1[:, i:i + 1])
            VOP("dve", "tensor_tensor", [b_k], [b_k], out=GT[:, i, :], in0=GT[:, i, :], in1=MK[:, i, :], op=ALU.mult)
            VOP("dve", "tensor_reduce", [b_k], [b_k], out=s1[:, 8 + i:9 + i], in_=GT[:, i, :], axis=AX.X, op=ALU.add)
        VOP("dve", "reciprocal", [b_k], [b_k], out=s1[:, 8:16], in_=s1[:, 8:16])
        for i in range(NQT):
            VOP("dve", "tensor_scalar", [b_k], [b_k], out=GT[:, i, :], in0=GT[:, i, :], scalar1=s1[:, 8 + i:9 + i], scalar2=None, op0=ALU.mult)
        VOP("dve", "tensor_copy", [b_k], [b_k], out=idxf[:], in_=idx8[:])
        VOP("dve", "tensor_copy", [b_k], [b_k], out=MKb[:], in_=MK[:])
        for i in range(NQT):
            out = PS[:, i * NE:(i + 1) * NE]
            for ip in range(i):
                MM(out, ones_b[:], MKb[:, ip, :], ip == 0, False, [b_k, cbuf], [pb[0]])
            MM(out, trsb[:], MKb[:, i, :], i == 0, True, [b_k], [pb[0]])
        VOP("dve", "tensor_copy", [], [b_k, pb[0]], out=POS[:].rearrange("p a b -> p (a b)"), in_=PS[:, 0:NQT * NE])
        for i in range(NQT):
            VOP("dve", "tensor_tensor", [b_k], [b_k], out=RF[:, i, :], in0=POS[:, i, :], in1=ecol[:], op=ALU.add)
        VOP("dve", "tensor_scalar", [b_k], [b_k], out=RF[:], in0=RF[:], scalar1=-float(ZROW), scalar2=None, op0=ALU.add)
        VOP("dve", "tensor_scalar", [b_k], [b_k], out=VL[:], in0=POS[:], scalar1=CAP - 0.5, scalar2=None, op0=ALU.is_lt)
        VOP("dve", "tensor_tensor", [b_k], [b_k], out=RF[:], in0=RF[:], in1=VL[:], op=ALU.mult)
        VOP("dve", "tensor_scalar", [b_k], [b_k], out=RF[:], in0=RF[:], scalar1=float(ZROW), scalar2=None, op0=ALU.add)
        for i in range(NQT):
            for k in range(4):
                VOP("dve", "tensor_scalar", [b_k], [b_k], out=oh[:], in0=iot[:, 0:NE], scalar1=idxf[:, i, k:k + 1], scalar2=None, op0=ALU.is_equal)
                VOP("dve", "tensor_tensor", [b_k], [b_k], out=oh[:], in0=oh[:], in1=RF[:, i, :], op=ALU.mult)
                VOP("dve", "tensor_reduce", [b_k], [b_k], out=rkf[:, i * 4 + k:i * 4 + k + 1], in_=oh[:], axis=AX.X, op=ALU.add)
        VOP("dve", "tensor_copy", [b_k], [b_rt], out=rki[:], in_=rkf[:])
        for i in range(NQT):
            for k in range(2):
                VOP("dve", "tensor_scalar", [b_k, cbuf], [b_k], out=R5[:, i, :, k], in0=ones_f[:, 0:NE], scalar1=tix[:, k * 8 + i:k * 8 + i + 1], scalar2=None, op0=ALU.mult)
        VOP("dve", "tensor_copy", [b_k], [b_k], out=gres[:], in_=GT[:])
        for k in range(3):
            VOP("dve", "tensor_copy", [b_k], [b_k], out=R5[:, :, :, 2 + k], in_=gres[:])
            if k < 2:
                VOP("dve", "tensor_copy", [b_k], [b_k], out=gtmp[:], in_=R5[:, :, :, 2 + k])
                VOP("dve", "tensor_tensor", [b_k], [b_k], out=gres[:], in0=gres[:], in1=gtmp[:], op=ALU.subtract)
        for e in range(NE):
            s = e % 2
            for i in range(NQT):
                VOP("dve", "tensor_scalar", [b_k], [Seb[s]], out=Se[s][:, i, :], in0=iot[:, 0:CAP], scalar1=POS[:, i, e:e + 1], scalar2=MK[:, i, e:e + 1],
                    op0=ALU.is_equal, op1=ALU.mult)
            for b in range(2):
                col = 512 + (e * 2 + b) * 5
                for i in range(NQT):
                    MM(PS[:, col:col + 5], Se[s][:, i, b * 128:(b + 1) * 128], R5[:, i, e, :], i == 0, i == NQT - 1, [Seb[s], b_k], [pb[1]])
        VOP("dve", "tensor_copy", [], [b_k, pb[1]], out=SI[:].rearrange("p a b -> p (a b)"), in_=PS[:, 512:512 + 320])
        VOP("dve", "scalar_tensor_tensor", [b_k], [b_k], out=vtmp[:], in0=SI[:, :, 0], scalar=32.0, in1=SI[:, :, 1], op0=ALU.mult, op1=ALU.add)
        VOP("dve", "tensor_scalar", [b_k], [b_k], out=gf[:], in0=vtmp[:], scalar1=-1.0, scalar2=0.0, op0=ALU.add, op1=ALU.max)
        VOP("dve", "tensor_copy", [b_k], [b_rt], out=gi[:], in_=gf[:])
        VOP("dve", "tensor_tensor", [b_k], [b_k], out=vtmp[:], in0=SI[:, :, 2], in1=SI[:, :, 3], op=ALU.add)
        VOP("dve", "tensor_tensor", [b_k], [b_rt], out=sg[:], in0=vtmp[:], in1=SI[:, :, 4], op=ALU.add)
        P.barrier()
    sL.close()
    if dbg and "rt_dbg" in dbg:
        d1_ = dscr("rt_dbg", [128, 64 + 64 + 32], F32)
        with contextlib.ExitStack() as sd:
            tdb = P.sbuf(sd, [128, 160], F32, "tdb")
            b_d = Buf()
            VOP("dve", "tensor_copy", [b_rt], [b_d], out=tdb[:, 0:64], in_=gi[:])
            VOP("dve", "tensor_copy", [b_rt], [b_d], out=tdb[:, 64:128], in_=sg[:])
            VOP("dve", "tensor_copy", [b_rt], [b_d], out=tdb[:, 128:160], in_=rki[:])
            P.dma("sp", d1_, tdb[:], [b_d], [], "dbg")
            P.barrier()
    if stop_after <= 10:
        return nc, P

    W1COLS, W2COLS = 3072, D
    with contextlib.ExitStack() as s11:
        wt1 = [P.sbuf(s11, [128, KC, 512], BF16, f"w1_{i}") for i in range(2)]
        wt1b = bufs(2)
        wt2 = [P.sbuf(s11, [128, 12, 512], BF16, f"w2_{i}") for i in range(2)]
        wt2b = bufs(2)
        xg = [P.sbuf(s11, [128, D], BF16, f"xg{i}") for i in range(2)]
        xgb = bufs(2)
        xgT = [P.sbuf(s11, [128, KC, 128], BF16, f"xgT{i}") for i in range(2)]
        xgTb = bufs(2)
        B1bc = P.sbuf(s11, [128, W1COLS], F32, "B1bc")
        B2bc = P.sbuf(s11, [128, D], F32, "B2bc")
        b_b1, b_b2 = Buf(), Buf()
        gact = [P.sbuf(s11, [128, 1536], F32, f"gact{i}") for i in range(2)]
        gab = bufs(2)
        act = [P.sbuf(s11, [128, 1536], BF16, f"act{i}") for i in range(2)]
        actb = bufs(2)
        actT = [P.sbuf(s11, [128, 12, 128], BF16, f"actT{i}") for i in range(2)]
        actTb = bufs(2)
        ot = [P.sbuf(s11, [128, 1024], F32, f"ot{i}") for i in range(2)]
        otb = bufs(2)
        tq = [P.sbuf(s11, [128, 512], F32, f"tq{i}") for i in range(3)]
        tqb = bufs(3)
        sgm = [P.sbuf(s11, [128, 512], F32, f"sgm{i}") for i in range(2)]
        sgmb = bufs(2)
        VOP("dve", "memset", [], [otb[0]], ot[0][:], 0.0)
        for q4 in range(4):
            P.dma("sp", ys_d[ZROW:ZROW + 128, q4 * 1024:(q4 + 1) * 1024], ot[0][:], [otb[0]], [], "s11z")
        groups = []
        for e in range(ne):
            for g in range(6):
                groups.append((1, e, g))
            for g in range(8):
                groups.append((2, e, g))

        def issue_load(n):
            kind, e, g = groups[n]
            if kind == 1:
                s = g % 2
                cast_load(wt1[s][:], I("w1")[e].rearrange("(kc p) n -> p kc n", p=128)[:, :, g * 512:(g + 1) * 512], [], [wt1b[s]], f"s11w1{s}")
            else:
                s = g % 2
                cast_load(wt2[s][:], I("w2")[e].rearrange("(kc p) n -> p kc n", p=128)[:, :, g * 512:(g + 1) * 512], [], [wt2b[s]], f"s11w2{s}")

        def pe_fill(k):
            for _ in range(k):
                MM(PS[:, 4 * 512:5 * 512], ident_b[:], kiT[:, 0:512], True, True, [cbuf], [pb[4]])

        issue_load(0)
        nb = 0
        nt = 0
        for n, (kind, e, g) in enumerate(groups):
            if kind == 1 and g == 0:
                P.dma("sp", B1bc[:], I("b1")[e:e + 1, :].partition_broadcast(128), [], [b_b1], "s11b1")
                P.dma("sp", B2bc[:], I("b2")[e:e + 1, :].partition_broadcast(128), [], [b_b2], "s11b2")
                for b in range(2):
                    k = e * 2 + b
                    P.op("pool", lambda en, b=b, k=k: en.indirect_dma_start(out=xg[b][:, :], out_offset=None, in_=u2_d[:, :],
                         in_offset=bass.IndirectOffsetOnAxis(ap=gi[:, k:k + 1], axis=0), bounds_check=None),
                         [b_rt], [xgb[b]], dma=True, chan=f"s11g{b}")
            if n + 1 < len(groups):
                issue_load(n + 1)
            if kind == 1 and g == 0:
                for b in range(2):
                    for f2 in range(2):
                        for j in range(16):
                            kc = f2 * 16 + j
                            TR(PSB[:, 4096 + j * 128:4096 + (j + 1) * 128], xg[b][:, kc * 128:(kc + 1) * 128], ident_b[:], [xgb[b], cbuf], [pb[4 + j // 8]])
                        evac(f2, xgT[b][:, f2 * 16:(f2 + 1) * 16, :], PSB[:, 4096:4096 + 2048], [xgTb[b], pb[4], pb[5]])
            if kind == 1:
                s = g % 2
                csl = slice(g * 512, (g + 1) * 512)
                for b in range(2):
                    bank = nb % 4
                    nb += 1
                    tqi = nt % 3
                    nt += 1
                    for kc in range(KC):
                        MM(ps(bank), xgT[b][:, kc, :], wt1[s][:, kc, :], kc == 0, kc == KC - 1, [xgTb[b], wt1b[s]], [pb[bank]])
                    VOP("dve", "tensor_tensor", [b_b1], [tqb[tqi], pb[bank]], out=tq[tqi][:], in0=ps(bank), in1=B1bc[:, csl], op=ALU.add)
                    if g < 3:
                        VOP("dve", "tensor_scalar", [tqb[tqi]], [tqb[tqi]], out=tq[tqi][:], in0=tq[tqi][:], scalar1=7.0, scalar2=None, op0=ALU.min)
                        sgi = nt % 2
                        ACT(sgm[sgi][:], tq[tqi][:], AF.Sigmoid, [tqb[tqi]], [sgmb[sgi]], scale=1.702)
                        VOP("dve", "tensor_tensor", [tqb[tqi], sgmb[sgi]], [gab[b]], out=gact[b][:, csl], in0=tq[tqi][:], in1=sgm[sgi][:], op=ALU.mult)
                    else:
                        c2 = slice((g - 3) * 512, (g - 2) * 512)
                        VOP("dve", "tensor_scalar", [tqb[tqi]], [tqb[tqi]], out=tq[tqi][:], in0=tq[tqi][:], scalar1=7.0, scalar2=-7.0, op0=ALU.min, op1=ALU.max)
                        VOP("dve", "scalar_tensor_tensor", [tqb[tqi], gab[b]], [actb[b]], out=act[b][:, c2], in0=tq[tqi][:], scalar=1.0, in1=gact[b][:, c2],
                            op0=ALU.add, op1=ALU.mult)
                pe_fill(28)
                if g == 5:
                    for b in range(2):
                        for j in range(12):
                            TR(PSB[:, 6144 + j * 128:6144 + (j + 1) * 128], act[b][:, j * 128:(j + 1) * 128], ident_b[:], [actb[b], cbuf], [pb[6 + j // 8]])
                        evac(b, actT[b][:].rearrange("p a b -> p (a b)"), PSB[:, 6144:6144 + 1536], [actTb[b], pb[6], pb[7]])
            else:
                s = g % 2
                csl = slice(g * 512, (g + 1) * 512)
                for b in range(2):
                    bank = nb % 4
                    nb += 1
                    tqi = nt % 3
                    nt += 1
                    for kc in range(12):
                        MM(ps(bank), actT[b][:, kc, :], wt2[s][:, kc, :], kc == 0, kc == 11, [actTb[b], wt2b[s]], [pb[bank]])
                    VOP("dve", "tensor_tensor", [b_b2], [tqb[tqi], pb[bank]], out=tq[tqi][:], in0=ps(bank), in1=B2bc[:, csl], op=ALU.add)
                    k = e * 2 + b
                    ACT(ot[b][:, (g % 2) * 512:(g % 2 + 1) * 512], tq[tqi][:], AF.Copy, [tqb[tqi], b_rt], [otb[b]], scale=sg[:, k:k + 1])
                    if g % 2 == 1:
                        P.dma("sp", ys_d[e * CAP + b * 128:e * CAP + (b + 1) * 128, (g // 2) * 1024:(g // 2 + 1) * 1024], ot[b][:], [otb[b]], [], f"s11o{b}")
                pe_fill(10)
        P.barrier()
    if stop_after <= 11:
        return nc, P

    with contextlib.ExitStack() as s12:
        GM = P.sbuf(s12, [128, D], F32, "GM")
        L2G = P.sbuf(s12, [128, D], F32, "L2G")
        L2B = P.sbuf(s12, [128, D], F32, "L2B")
        yk = [P.sbuf(s12, [128, D], F32, f"yk{i}") for i in range(4)]
        ykb = bufs(4)
        x1t = P.sbuf(s12, [128, D], F32, "x1t12")
        tmp = P.sbuf(s12, [128, D], F32, "tmp12")
        st = P.sbuf(s12, [128, 4], F32, "st12")
        b_c, b_x, b_t, b_st = Buf(), Buf(), Buf(), Buf()
        P.dma("sp", GM[:], mod_d[:, 5 * D:6 * D].partition_broadcast(128), [], [b_c], "s12a")
        P.dma("sp", L2G[:], I("ln2_g").partition_broadcast(128), [], [b_c], "s12b")
        P.dma("sp", L2B[:], I("ln2_b").partition_broadcast(128), [], [b_c], "s12a")
        outs = []
        for i in range(NQT):
            rsl = slice(i * 128, (i + 1) * 128)
            P.dma("sp", x1t[:], x1_d[rsl, :], [], [b_x], "s12x")
            for k in range(4):
                c = i * 4 + k
                P.op("pool", lambda en, k=k, c=c: en.indirect_dma_start(out=yk[k][:, :], out_offset=None, in_=ys_d[:, :],
                     in_offset=bass.IndirectOffsetOnAxis(ap=rki[:, c:c + 1], axis=0), bounds_check=None),
                     [b_rt], [ykb[k]], dma=True, chan=f"s12g{k}")
            VOP("dve", "tensor_tensor", [ykb[0], ykb[1]], [ykb[0]], out=yk[0][:], in0=yk[0][:], in1=yk[1][:], op=ALU.add)
            VOP("dve", "tensor_tensor", [ykb[2], ykb[3]], [ykb[2]], out=yk[2][:], in0=yk[2][:], in1=yk[3][:], op=ALU.add)
            VOP("dve", "tensor_tensor", [ykb[0], ykb[2]], [ykb[0]], out=yk[0][:], in0=yk[0][:], in1=yk[2][:], op=ALU.add)
            VOP("dve", "tensor_tensor", [ykb[0], b_c], [ykb[0]], out=yk[0][:], in0=yk[0][:], in1=GM[:], op=ALU.mult)
            VOP("dve", "scalar_tensor_tensor", [b_x, ykb[0]], [b_x], out=x1t[:], in0=x1t[:], scalar=ALPHA, in1=yk[0][:], op0=ALU.mult, op1=ALU.add)
            VOP("dve", "tensor_reduce", [b_x], [b_st], out=st[:, 0:1], in_=x1t[:], axis=AX.X, op=ALU.add)
            VOP("dve", "tensor_scalar", [b_st], [b_st], out=st[:, 1:2], in0=st[:, 0:1], scalar1=-1.0 / D, scalar2=None, op0=ALU.mult)
            ACT(tmp[:], x1t[:], AF.Square, [b_x, b_st], [b_t], bias=st[:, 1:2])
            VOP("dve", "tensor_reduce", [b_t], [b_st], out=st[:, 2:3], in_=tmp[:], axis=AX.X, op=ALU.add)
            VOP("dve", "tensor_scalar", [b_st], [b_st], out=st[:, 2:3], in0=st[:, 2:3], scalar1=1.0 / D, scalar2=1e-5, op0=ALU.mult, op1=ALU.add)
            RSQRT(st[:, 2:3], b_st)
            VOP("dve", "tensor_scalar", [b_x, b_st], [b_t], out=tmp[:], in0=x1t[:], scalar1=st[:, 1:2], scalar2=st[:, 2:3], op0=ALU.add, op1=ALU.mult)
            VOP("dve", "tensor_tensor", [b_c], [b_t], out=tmp[:], in0=tmp[:], in1=L2G[:], op=ALU.mult)
            VOP("dve", "tensor_tensor", [b_c], [b_t], out=tmp[:], in0=tmp[:], in1=L2B[:], op=ALU.add)
            outs.append(P.dma("sp", y_out[rsl, :], tmp[:], [b_t], [], "s12o"))
        P.barrier()
    sR.close()
    return nc, P


def _t5_bucket(dist):
    import math
    d = np.maximum(dist.astype(np.float32), np.float32(1.0))
    large = 16 + (np.log(d / np.float32(16)) / np.float32(math.log(128 / 16)) * np.float32(16)).astype(np.int32)
    large = np.minimum(large, 31)
    return np.where(dist < 16, dist, large)


def _fm(v, n):
    return np.ascontiguousarray(np.asarray(v, np.float32).reshape(n, 128).T)


def prep_core(inp, core, ne=NE):
    b, par = core // 2, core % 2
    f = lambda a: np.ascontiguousarray(np.asarray(a, np.float32))
    x = f(inp["x"][b])
    own = np.concatenate([x[(2 * i + par) * 128:(2 * i + par + 1) * 128] for i in range(NQT)], axis=0)
    p = np.arange(128)[:, None]
    n = np.arange(2304)[None, :]
    dist = np.maximum(par * 128 + p + 2048 - n, 0)
    bidx = _t5_bucket(dist)
    rb = f(inp["rel_bias"])
    biasG = np.ascontiguousarray(np.transpose(rb[bidx], (2, 0, 1)))
    esel = np.zeros((80, 16, 128), np.float32)
    for h in range(16):
        esel[h, h, :] = 1; esel[32 + h, h, :] = 1; esel[64 + h, h, :] = 1
    masked = (np.arange(128)[None, :] > np.arange(128)[:, None]).astype(np.float32)
    if par == 0:
        maskc = np.concatenate([masked, np.ones((128, 128), np.float32)], axis=1)
    else:
        maskc = np.concatenate([np.zeros((128, 128), np.float32), masked], axis=1)
    m = {
        "x_all": x, "x_own": f(own), "cT": _fm(inp["c"][b], KC),
        "w_ada": f(inp["w_ada"][0]), "b_ada": f(inp["b_ada"][0]).reshape(1, -1),
        "w_in": f(inp["w_in"][0]), "b_forget": f(inp["b_forget"][0]).reshape(1, 16),
        "qn_g": _fm(inp["q_norm_g"][0], 12), "kvn_g": _fm(inp["kv_norm_g"][0], 4),
        "ki_gb": np.ascontiguousarray(np.stack([f(inp["kidx_ln_g"][0]), f(inp["kidx_ln_b"][0])], axis=1)),
        "w_uq": f(inp["w_uq"][0]), "w_uk": f(inp["w_uk"][0]).reshape(512, 2048), "w_uv": f(inp["w_uv"][0]).reshape(512, 2048),
        "w_iq": f(inp["w_iq"][0]),
        "mg_g": _fm(np.concatenate([f(inp["fox_out_g"][0]), f(inp["dsa_out_g"][0])]), KC),
        "w_o": f(inp["w_o"][0]), "ln1_g": f(inp["ln1_g"][0]).reshape(1, -1), "ln1_b": f(inp["ln1_b"][0]).reshape(1, -1),
        "w_router": f(inp["w_router"][0]), "b_router": f(inp["b_router"][0]).reshape(1, -1),
        "w1": f(inp["w1"][0][:ne]), "b1": f(inp["b1"][0][:ne]), "w2": f(inp["w2"][0][:ne]), "b2": f(inp["b2"][0][:ne]),
        "ln2_g": f(inp["ln2_g"][0]).reshape(1, -1), "ln2_b": f(inp["ln2_b"][0]).reshape(1, -1),
        "biasG": biasG, "ident": np.eye(128, dtype=np.float32), "esel": esel.reshape(80, 2048),
        "maskc": maskc, "triu": (np.arange(128)[:, None] < np.arange(128)[None, :]).astype(np.float32),
        "iota": np.tile(np.arange(256, dtype=np.float32)[None, :], (128, 1)),
        "tidx": np.concatenate([(np.arange(8)[None, :] * 128 + np.arange(128)[:, None] + 1) // 32,
                                (np.arange(8)[None, :] * 128 + np.arange(128)[:, None] + 1) % 32], axis=1).astype(np.float32),
    }
    return m


def kernel(**inputs):
    nc, P = build()
    P.emit()
    n = 8
    in_maps = [prep_core(inputs, c) for c in range(n)]
    res = run_bass_kernel_spmd(nc, in_maps, core_ids=list(range(n)))
    out = np.empty((4, S, D), np.float32)
    for c in range(n):
        b, par = c // 2, c % 2
        y = np.asarray(res.results[c]["y"])
        for i in range(NQT):
            out[b, (2 * i + par) * 128:(2 * i + par + 1) * 128, :] = y[i * 128:(i + 1) * 128, :]
    return out
```

```python
import contextlib
import numpy as np
import concourse.bass as bass
import concourse.mybir as mybir
from concourse.bass_utils import run_bass_kernel_spmd

F32 = mybir.dt.float32
BF16 = mybir.dt.bfloat16
I32 = mybir.dt.int32
U32 = mybir.dt.uint32
ALU = mybir.AluOpType
AF = mybir.ActivationFunctionType
AX = mybir.AxisListType

QUEUES = ("pe", "act", "dve", "pool", "sp")


class Buf:
    __slots__ = ("name", "last_w", "readers")

    def __init__(self, name="b"):
        self.name = name
        self.last_w = None
        self.readers = []


def bufs(n, name="b"):
    return [Buf(f"{name}{i}") for i in range(n)]


class Op:
    __slots__ = ("idx", "q", "fn", "deps", "is_dma", "chan", "signal", "ticket", "sem")

    def __init__(self, idx, q, fn, is_dma, chan):
        self.idx = idx
        self.q = q
        self.fn = fn
        self.deps = []
        self.is_dma = is_dma
        self.chan = chan
        self.signal = False
        self.ticket = None
        self.sem = None


class Prog:
    def __init__(self, nc):
        self.nc = nc
        self.ops = []
        self.chan_last = {}
        self.chan_map = {}
        self.last_op = {q: None for q in QUEUES}
        self.stack = contextlib.ExitStack()
        self.nsb = 0

    def sbuf(self, st, shape, dtype, name=None):
        self.nsb += 1
        return st.enter_context(self.nc.sbuf_tensor(f"{name or 'sb'}_{self.nsb}", list(shape), dtype))

    def op(self, q, fn, reads=(), writes=(), dma=False, chan=None, extra=()):
        o = Op(len(self.ops), q, fn, dma, chan)
        deps = {}
        for b in reads:
            if b.last_w is not None:
                deps[b.last_w.idx] = b.last_w
        for b in writes:
            if b.last_w is not None:
                deps[b.last_w.idx] = b.last_w
            for r in b.readers:
                deps[r.idx] = r
        for d in extra:
            if d is not None:
                deps[d.idx] = d
        if dma:
            chan = self.chan_map.setdefault(chan, f"C{len(self.chan_map)}")
            o.chan = chan
            prev = self.chan_last.get(chan)
            if prev is not None:
                deps[prev.idx] = prev
            self.chan_last[chan] = o
        for d in deps.values():
            if (not d.is_dma) and (not dma) and d.q == q and q == "pe":
                continue
            o.deps.append(d)
            d.signal = True
        rk = ("dma", chan) if dma else q
        for b in reads:
            b.readers = [r for r in b.readers if (("dma", r.chan) if r.is_dma else r.q) != rk] + [o]
        for b in writes:
            b.last_w = o
            b.readers = []
        if not dma:
            self.last_op[q] = o
        self.ops.append(o)
        return o

    def dma(self, q, out, in_, reads, writes, chan, **kw):
        return self.op(q, lambda e: e.dma_start(out=out, in_=in_, **kw), reads, writes, dma=True, chan=chan)

    def barrier(self):
        pend = [d for d in self.chan_last.values()] + [d for d in self.last_op.values() if d is not None]
        for q in QUEUES:
            self.op(q, lambda e: e.nop(), extra=pend)
        self.chan_map = {}

    def emit(self):
        nc = self.nc
        st = self.stack
        qsem = {q: st.enter_context(nc.semaphore(f"q_{q}")) for q in QUEUES}
        chans = {}
        for o in self.ops:
            if o.is_dma and o.chan not in chans:
                chans[o.chan] = st.enter_context(nc.semaphore(f"c_{len(chans)}"))
        qcount = {q: 0 for q in QUEUES}
        ccount = {c: 0 for c in chans}
        for o in self.ops:
            if o.is_dma:
                ccount[o.chan] += 16
                o.sem = chans[o.chan]
                o.ticket = ccount[o.chan]
            elif o.signal:
                qcount[o.q] += 1
                o.sem = qsem[o.q]
                o.ticket = qcount[o.q]
        self.stats = dict(qcount=qcount, nchan=len(chans), maxc=max(list(ccount.values()) + [0]), nops=len(self.ops))
        byq = {q: [o for o in self.ops if o.q == q] for q in QUEUES}
        blk = st.enter_context(nc.Block())

        def run(q):
            def body(e):
                waited = {}
                for o in byq[q]:
                    need = {}
                    for d in o.deps:
                        k = id(d.sem)
                        if k not in need or need[k][1] < d.ticket:
                            need[k] = (d.sem, d.ticket)
                    for k, (s, v) in need.items():
                        if waited.get(k, 0) >= v:
                            continue
                        e.wait_ge(s, v)
                        waited[k] = v
                    try:
                        ins = o.fn(e)
                    except Exception:
                        print("EMIT FAIL op", o.idx, o.q, o.is_dma, o.chan, "deps", [(d.idx, d.q, d.ticket) for d in o.deps], flush=True)
                        raise
                    if o.is_dma:
                        ins.then_inc(o.sem, 16)
                    elif o.signal:
                        ins.then_inc(o.sem, 1)
            return body

        blk.tensor(run("pe"))
        blk.scalar(run("act"))
        blk.vector(run("dve"))
        blk.gpsimd(run("pool"))
        blk.sync(run("sp"))


D = 4096
KC = 32
S = 2048
T = 1024
NQT = 8
NE = 32
CAP = 256
ALPHA = 2.0 ** 0.25
SCALE = 128.0 ** -0.5
C_QF, C_KF, C_VF, C_F, C_CQ, C_CKV, C_KI, C_WI = 0, 2048, 4096, 6144, 6160, 7696, 8208, 8336
NEGBIG = -3.0e5
ZROW = NE * CAP


def build(stop_after=99, dbg=False, ne=NE):
    nc = bass.Bass("TRN2", target_bir_lowering=False)
    P = Prog(nc)
    G = P.stack

    def din(name, shape, dt=F32):
        return nc.dram_tensor(name, list(shape), dt, kind="ExternalInput").ap()

    def dscr(name, shape, dt):
        if dbg and name in dbg:
            return nc.dram_tensor(name, list(shape), dt, kind="ExternalOutput").ap()
        return nc.dram_tensor(name, list(shape), dt).ap()

    _specs = {
        "x_all": [S, D],
        "x_own": [T, D],
        "cT": [128, KC],
        "w_ada": [D, 6 * D],
        "b_ada": [1, 6 * D],
        "w_in": [D, 8368],
        "b_forget": [1, 16],
        "qn_g": [128, 12],
        "kvn_g": [128, 4],
        "ki_gb": [128, 2],
        "w_uq": [1536, 2048],
        "w_uk": [512, 2048],
        "w_uv": [512, 2048],
        "w_iq": [1536, 4096],
        "mg_g": [128, KC],
        "w_o": [D, D],
        "ln1_g": [1, D],
        "ln1_b": [1, D],
        "w_router": [D, NE],
        "b_router": [1, NE],
        "w1": [ne, D, 3072],
        "b1": [ne, 3072],
        "w2": [ne, 1536, D],
        "b2": [ne, D],
        "ln2_g": [1, D],
        "ln2_b": [1, D],
        "biasG": [16, 128, 2304],
        "ident": [128, 128],
        "esel": [80, 2048],
        "maskc": [128, 256],
        "triu": [128, 128],
        "iota": [128, 256],
        "tidx": [128, 16],
    }
    _ins = {}

    def I(name):
        if name not in _ins:
            _ins[name] = din(name, _specs[name])
        return _ins[name]

    y_out = nc.dram_tensor("y", [T, D], F32, kind="ExternalOutput").ap()

    mod_d = dscr("mod_d", [1, 6 * D], F32)
    kT_d = dscr("kT_d", [16, 128, S], BF16)
    v_d = dscr("v_d", [S, 2048], BF16)
    flog_d = dscr("flog_d", [S, 16], F32)
    ckvT_d = dscr("ckvT_d", [4, 128, S], F32)
    kidxT_d = dscr("kidxT_d", [128, S], F32)
    qT_d = dscr("qT_d", [16, 128, T], BF16)
    cqT_d = dscr("cqT_d", [12, 128, T], F32)
    widx_d = dscr("widx_d", [T, 32], F32)
    kdT_d = dscr("kdT_d", [16, 128, S], BF16)
    vd_d = dscr("vd_d", [S, 2048], BF16)
    qdT_d = dscr("qdT_d", [16, 128, T], BF16)
    qiT_d = dscr("qiT_d", [32, 128, T], BF16)
    sel_d = dscr("sel_d", [NQT, 128, S], BF16)
    r_d = dscr("r_d", [T, D], F32)
    x1_d = dscr("x1_d", [T, D], F32)
    u2_d = dscr("u2_d", [T, D], BF16)
    ys_d = dscr("ys_d", [NE * CAP + 128, D], F32)
    attn_d = dscr("attn_d", [T, D], BF16)

    PS = G.enter_context(nc.psum_tensor("PS", [128, 4096], F32))
    pb = bufs(8, "pb")
    PSB = PS[:].bitcast(BF16)

    def ps(b0, nb=1, rows=128, cols=None):
        c = cols if cols is not None else nb * 512
        return PS[0:rows, b0 * 512:b0 * 512 + c]

    ident_f = P.sbuf(G, [128, 128], F32, "identf")
    ident_b = P.sbuf(G, [128, 128], BF16, "identb")
    ones_f = P.sbuf(G, [128, 128], F32, "onesf")
    ones_b = P.sbuf(G, [128, 128], BF16, "onesb")
    modT = P.sbuf(G, [128, 192], F32, "modT")
    scA1 = P.sbuf(G, [128, KC], F32, "scA1")
    CS = P.sbuf(G, [80, S], BF16, "CS")
    kiT = P.sbuf(G, [128, S], BF16, "kiT")
    Esel = P.sbuf(G, [80, 2048], BF16, "Esel")
    maskb = P.sbuf(G, [128, 256], BF16, "maskb")
    maskbig = P.sbuf(G, [128, 256], F32, "maskbig")
    cbuf = Buf("consts")

    def MM(out, lhsT, rhs, start, stop, R, W, **kw):
        return P.op("pe", lambda e: e.matmul(out, lhsT=lhsT, rhs=rhs, start=start, stop=stop, **kw), R, W)

    def TR(out, in_, idn, R, W):
        return P.op("pe", lambda e: e.transpose(out, in_, idn), R, W)

    def ACT(out, in_, func, R, W, **kw):
        return P.op("act", lambda e: e.activation(out=out, in_=in_, func=func, **kw), R, W)

    def VOP(q, meth, R, W, *a, **kw):
        return P.op(q, lambda e: getattr(e, meth)(*a, **kw), R, W)

    def cast_load(dst, src, R, W, chan):
        return P.dma("pool", dst, src, R, W, chan)

    P.dma("sp", ident_f[:], I("ident"), [], [cbuf], "c0")
    VOP("dve", "tensor_copy", [cbuf], [cbuf], out=ident_b[:], in_=ident_f[:])
    VOP("dve", "memset", [], [cbuf], ones_f[:], 1.0)
    VOP("dve", "memset", [], [cbuf], ones_b[:], 1.0)
    cast_load(Esel[:], I("esel"), [], [cbuf], "c1")
    with contextlib.ExitStack() as s0:
        mtmp = P.sbuf(s0, [128, 256], F32, "mtmp")
        P.dma("sp", mtmp[:], I("maskc"), [], [cbuf], "c0")
        VOP("dve", "tensor_scalar", [cbuf], [cbuf], out=maskb[:], in0=mtmp[:], scalar1=NEGBIG, scalar2=None, op0=ALU.mult)
        VOP("dve", "tensor_scalar", [cbuf], [cbuf], out=maskbig[:], in0=mtmp[:], scalar1=-1.0e30, scalar2=None, op0=ALU.mult)
        P.barrier()

    sil = P.sbuf(G, [128, KC], BF16, "sil")
    b_sil = Buf("sil")
    wv_ada = I("w_ada").rearrange("(kc p) n -> p kc n", p=128)
    N_S0 = 24

    def mod_issue(g, wt_t, wt_b, brow_t, brow_b, tag):
        cast_load(wt_t[:], wv_ada[:, :, g * 512:(g + 1) * 512], [], [wt_b], f"ada{tag}")
        P.dma("sp", brow_t[:], I("b_ada")[:, g * 512:(g + 1) * 512], [], [brow_b], f"abr{tag}")

    def mod_compute(g, wt_t, wt_b, brow_t, brow_b, row_t, row_b, bank, tag, transpose):
        for kc in range(KC):
            MM(ps(bank, rows=1), sil[:, kc:kc + 1], wt_t[:, kc, :], kc == 0, kc == KC - 1, [b_sil, wt_b], [pb[bank]])
        VOP("dve", "tensor_tensor", [brow_b], [row_b, pb[bank]], out=row_t[:], in0=ps(bank, rows=1), in1=brow_t[:], op=ALU.add)
        P.dma("sp", mod_d[:, g * 512:(g + 1) * 512], row_t[:], [row_b], [], f"aro{tag}")
        if transpose:
            for j in range(4):
                c = g * 4 + j
                MM(PS[:, 1024 + c:1024 + c + 1], row_t[0:1, j * 128:(j + 1) * 128], ones_f[0:1, 0:1], True, True, [row_b, cbuf], [pb[2]])

    with contextlib.ExitStack() as s0:
        ct = P.sbuf(s0, [128, KC], F32, "ct")
        wt = [P.sbuf(s0, [128, KC, 512], BF16, f"wada{i}") for i in range(3)]
        wtb = bufs(3, "wada")
        row = [P.sbuf(s0, [1, 512], F32, f"row{i}") for i in range(2)]
        rowb = bufs(2, "row")
        brow = [P.sbuf(s0, [1, 512], F32, f"brow{i}") for i in range(2)]
        browb = bufs(2, "brow")
        P.dma("sp", ct[:], I("cT"), [], [b_sil], "s0a")
        ACT(sil[:], ct[:], AF.Silu, [b_sil], [b_sil])
        for g in range(N_S0):
            s = g % 3
            r2 = g % 2
            mod_issue(g, wt[s], wtb[s], brow[r2], browb[r2], f"{s}{r2}")
            mod_compute(g, wt[s], wtb[s], brow[r2], browb[r2], row[r2], rowb[r2], r2, f"{r2}", g < 16)
        b_mod = Buf("modT")
        VOP("dve", "tensor_copy", [], [b_mod, pb[2]], out=modT[:, 0:64], in_=PS[:, 1024:1024 + 64])
        VOP("dve", "tensor_scalar", [b_mod], [b_mod], out=scA1[:], in0=modT[:, 32:64], scalar1=1.0, scalar2=None, op0=ALU.add)
        P.barrier()
    if stop_after <= 0:
        return nc, P

    def proj_group(xsrc, tok0, kside):
        with contextlib.ExitStack() as sg:
            uT = P.sbuf(sg, [128, KC, T], BF16, "uT")
            with contextlib.ExitStack() as s1:
                xt = [P.sbuf(s1, [128, D], F32, f"xt{i}") for i in range(2)]
                xtb = bufs(2, "xt")
                ub = Buf("uT")
                n = 0
                for tt in range(8):
                    s = tt % 2
                    P.dma("sp", xt[s][:], xsrc[tt * 128:(tt + 1) * 128, :], [], [xtb[s]], f"xt{s}")
                    for k4 in range(8):
                        bank = n % 8
                        n += 1
                        for j in range(4):
                            kc = k4 * 4 + j
                            pso = PS[:, bank * 512 + j * 128: bank * 512 + (j + 1) * 128]
                            TR(pso, xt[s][:, kc * 128:(kc + 1) * 128], ident_f[:], [xtb[s], cbuf], [pb[bank]])
                        for j in range(4):
                            kc = k4 * 4 + j
                            pso = PS[:, bank * 512 + j * 128: bank * 512 + (j + 1) * 128]
                            dst = uT[:, kc, tt * 128:(tt + 1) * 128]
                            if bank % 2 == 0:
                                ACT(dst, pso, AF.Identity, [], [pb[bank]], scale=scA1[:, kc:kc + 1], bias=modT[:, kc:kc + 1])
                            else:
                                VOP("dve", "tensor_scalar", [], [pb[bank]], out=dst, in0=pso, scalar1=scA1[:, kc:kc + 1],
                                    scalar2=modT[:, kc:kc + 1], op0=ALU.mult, op1=ALU.add)
                P.barrier()
                if dbg and "uT_d" in dbg and tok0 == 0 and kside:
                    uT_d = dscr("uT_d", [128, KC, T], BF16)
                    P.dma("sp", uT_d, uT[:], [], [], "dbg")
                    P.barrier()
                if stop_after <= 1:
                    return
            with contextlib.ExitStack() as s2:
                wt = [P.sbuf(s2, [128, KC, 512], BF16, f"win{i}") for i in range(2)]
                wtb = bufs(2, "win")
                stg = [P.sbuf(s2, [128, T], F32, f"stg{i}") for i in range(2)]
                stgb = bufs(2, "stg")
                wv = I("w_in").rearrange("(kc p) n -> p kc n", p=128)
                cnt = {"g": 0, "t": 0}

                def fm_group(c0, ncols, epi):
                    s = cnt["g"] % 2
                    cnt["g"] += 1
                    cast_load(wt[s][:, :, 0:ncols], wv[:, :, c0:c0 + ncols], [], [wtb[s]], f"win{s}")
                    for j in range((ncols + 127) // 128):
                        m = min(128, ncols - j * 128)
                        pr = cnt["t"] % 4
                        cnt["t"] += 1
                        for tc in range(2):
                            bank = pr * 2 + tc
                            for kc in range(KC):
                                MM(PS[0:m, bank * 512:(bank + 1) * 512], wt[s][:, kc, j * 128:j * 128 + m],
                                   uT[:, kc, tc * 512:(tc + 1) * 512], kc == 0, kc == KC - 1, [wtb[s]], [pb[bank]])
                        epi(j, PS[0:m, pr * 1024:(pr + 1) * 1024], [pb[pr * 2], pb[pr * 2 + 1]])

                def epi_store(dst_fn, bf):
                    def epi(j, pap, pbs):
                        s = cnt["t"] % 2
                        m = pap.shape[0]
                        if bf:
                            o = stg[s][:].bitcast(BF16)[0:m, 0:T]
                        else:
                            o = stg[s][0:m, :]
                        if cnt["t"] % 2 == 0:
                            ACT(o, pap, AF.Copy, [], [stgb[s]] + pbs)
                        else:
                            VOP("dve", "tensor_copy", [], [stgb[s]] + pbs, out=o, in_=pap)
                        P.dma("sp", dst_fn(j), o, [stgb[s]], [], f"stgo{s}")
                    return epi

                if kside:
                    for g4 in range(4):
                        fm_group(C_KF + g4 * 512, 512, epi_store(lambda j, g4=g4: kT_d[g4 * 4 + j, :, tok0:tok0 + T], True))
                    fm_group(C_CKV, 512, epi_store(lambda j: ckvT_d[j, :, tok0:tok0 + T], False))
                    fm_group(C_KI, 128, epi_store(lambda j: kidxT_d[:, tok0:tok0 + T], False))
                else:
                    for g4 in range(4):
                        fm_group(C_QF + g4 * 512, 512, epi_store(lambda j, g4=g4: qT_d[g4 * 4 + j, :, :], True))
                    for g3 in range(3):
                        fm_group(C_CQ + g3 * 512, 512, epi_store(lambda j, g3=g3: cqT_d[g3 * 4 + j, :, :], False))

                def tm_group(c0, ncols, dst_fn, bf):
                    s = cnt["g"] % 2
                    cnt["g"] += 1
                    cast_load(wt[s][:, :, 0:ncols], wv[:, :, c0:c0 + ncols], [], [wtb[s]], f"win{s}")
                    for tt in range(8):
                        bank = cnt["t"] % 8
                        s2_ = cnt["t"] % 2
                        cnt["t"] += 1
                        for kc in range(KC):
                            MM(PS[:, bank * 512:bank * 512 + ncols], uT[:, kc, tt * 128:(tt + 1) * 128], wt[s][:, kc, 0:ncols],
                               kc == 0, kc == KC - 1, [wtb[s]], [pb[bank]])
                        if bf:
                            o = stg[s2_][:].bitcast(BF16)[:, 0:ncols]
                        else:
                            o = stg[s2_][:, 0:ncols]
                        pap = PS[:, bank * 512:bank * 512 + ncols]
                        if tt % 2 == 0:
                            ACT(o, pap, AF.Copy, [], [stgb[s2_], pb[bank]])
                        else:
                            VOP("dve", "tensor_copy", [], [stgb[s2_], pb[bank]], out=o, in_=pap)
                        P.dma("sp", dst_fn(tt), o, [stgb[s2_]], [], f"stgo{s2_}")

                if kside:
                    for g4 in range(4):
                        tm_group(C_VF + g4 * 512, 512, lambda tt, g4=g4: v_d[tok0 + tt * 128:tok0 + (tt + 1) * 128, g4 * 512:(g4 + 1) * 512], True)
                    tm_group(C_F, 16, lambda tt: flog_d[tok0 + tt * 128:tok0 + (tt + 1) * 128, :], False)
                else:
                    tm_group(C_WI, 32, lambda tt: widx_d[tt * 128:(tt + 1) * 128, :], False)
                P.barrier()

    proj_group(I("x_all")[0:T, :], 0, True)
    if stop_after <= 1:
        return nc, P
    proj_group(I("x_all")[T:S, :], T, True)
    proj_group(I("x_own"), 0, False)
    if stop_after <= 2:
        return nc, P

    def RSQRT(t, B):
        ACT(t, t, AF.Sqrt, [B], [B])
        VOP("dve", "reciprocal", [B], [B], out=t, in_=t)

    def evac(i, out, in_, W):
        if i % 2 == 0:
            return ACT(out, in_, AF.Copy, [], W)
        return VOP("dve", "tensor_copy", [], W, out=out, in_=in_)

    with contextlib.ExitStack() as s3:
        ckvT = P.sbuf(s3, [128, 4, S], F32, "ckvT")
        ckvn = P.sbuf(s3, [128, 4, S], BF16, "ckvn")
        sq = P.sbuf(s3, [128, 4, 512], F32, "sq")
        rstd = P.sbuf(s3, [128, 512], F32, "rstd")
        kvg = P.sbuf(s3, [128, 4], F32, "kvg")
        wuk = P.sbuf(s3, [128, 4, 2048], BF16, "wuk")
        wuv = P.sbuf(s3, [128, 4, 2048], BF16, "wuv")
        stg = [P.sbuf(s3, [128, S], BF16, f"s3stg{i}") for i in range(2)]
        stgb = bufs(2)
        b_ck, b_w, b_n, b_sq, b_rs = Buf(), Buf(), Buf(), Buf(), Buf()
        for r in range(4):
            P.dma("sp", ckvT[:, r, :], ckvT_d[r], [], [b_ck], f"s3l{r % 2}")
        P.dma("sp", kvg[:], I("kvn_g"), [], [b_ck], "s3g")
        cast_load(wuk[:], I("w_uk").rearrange("(r p) n -> p r n", p=128), [], [b_w], "s3w0")
        cast_load(wuv[:], I("w_uv").rearrange("(r p) n -> p r n", p=128), [], [b_w], "s3w1")
        for tc in range(4):
            sl = slice(tc * 512, (tc + 1) * 512)
            bank = tc % 2
            for r in range(4):
                ACT(sq[:, r, :], ckvT[:, r, sl], AF.Square, [b_ck], [b_sq])
            for r in range(4):
                MM(ps(bank), ones_f[:], sq[:, r, :], r == 0, r == 3, [b_sq, cbuf], [pb[bank]])
            VOP("dve", "tensor_scalar", [], [b_rs, pb[bank]], out=rstd[:], in0=ps(bank), scalar1=1.0 / 512, scalar2=1e-6, op0=ALU.mult, op1=ALU.add)
            RSQRT(rstd[:], b_rs)
            for r in range(4):
                VOP("dve", "scalar_tensor_tensor", [b_ck, b_rs], [b_n], out=ckvn[:, r, sl], in0=ckvT[:, r, sl], scalar=kvg[:, r:r + 1],
                    in1=rstd[:], op0=ALU.mult, op1=ALU.mult)
        for h in range(16):
            half = h % 2
            for tc in range(4):
                bank = half * 4 + tc
                for r in range(4):
                    MM(ps(bank), wuk[:, r, h * 128:(h + 1) * 128], ckvn[:, r, tc * 512:(tc + 1) * 512], r == 0, r == 3, [b_w, b_n], [pb[bank]])
            evac(half, stg[half][:], PS[:, half * 2048:(half + 1) * 2048], [stgb[half]] + pb[half * 4:half * 4 + 4])
            P.dma("sp", kdT_d[h], stg[half][:], [stgb[half]], [], f"s3o{half}")
        for st_ in range(16):
            half = st_ % 2
            for cg in range(4):
                bank = half * 4 + cg
                for r in range(4):
                    MM(ps(bank), ckvn[:, r, st_ * 128:(st_ + 1) * 128], wuv[:, r, cg * 512:(cg + 1) * 512], r == 0, r == 3, [b_w, b_n], [pb[bank]])
            evac(half, stg[half][:], PS[:, half * 2048:(half + 1) * 2048], [stgb[half]] + pb[half * 4:half * 4 + 4])
            P.dma("sp", vd_d[st_ * 128:(st_ + 1) * 128, :], stg[half][:], [stgb[half]], [], f"s3o{half}")
        P.barrier()
    with contextlib.ExitStack() as s3:
        kx = P.sbuf(s3, [128, S], F32, "kx")
        kg = P.sbuf(s3, [128, 2], F32, "kg")
        sqk = P.sbuf(s3, [128, 512], F32, "sqk")
        mean = P.sbuf(s3, [128, 512], F32, "mean")
        var = P.sbuf(s3, [128, 512], F32, "var")
        xm = P.sbuf(s3, [128, 512], F32, "xm")
        b_kx, b_t = Buf(), Buf()
        P.dma("sp", kx[:], kidxT_d, [], [b_kx], "s3k")
        P.dma("sp", kg[:], I("ki_gb"), [], [b_kx], "s3kg")
        for tc in range(4):
            sl = slice(tc * 512, (tc + 1) * 512)
            ACT(sqk[:], kx[:, sl], AF.Square, [b_kx], [b_t])
            MM(ps(0), ones_f[:], kx[:, sl], True, True, [b_kx, cbuf], [pb[0]])
            MM(ps(1), ones_f[:], sqk[:], True, True, [b_t, cbuf], [pb[1]])
            VOP("dve", "tensor_scalar", [], [b_t, pb[0]], out=mean[:], in0=ps(0), scalar1=1.0 / 128, scalar2=None, op0=ALU.mult)
            VOP("dve", "tensor_scalar", [], [b_t, pb[1]], out=var[:], in0=ps(1), scalar1=1.0 / 128, scalar2=None, op0=ALU.mult)
            VOP("dve", "tensor_tensor", [b_t], [b_t], out=xm[:], in0=mean[:], in1=mean[:], op=ALU.mult)
            VOP("dve", "tensor_tensor", [b_t], [b_t], out=var[:], in0=var[:], in1=xm[:], op=ALU.subtract)
            VOP("dve", "tensor_scalar", [b_t], [b_t], out=var[:], in0=var[:], scalar1=1e-5, scalar2=None, op0=ALU.add)
            RSQRT(var[:], b_t)
            VOP("dve", "tensor_tensor", [b_t, b_kx], [b_t], out=xm[:], in0=kx[:, sl], in1=mean[:], op=ALU.subtract)
            VOP("dve", "tensor_tensor", [b_t], [b_t], out=xm[:], in0=xm[:], in1=var[:], op=ALU.mult)
            VOP("dve", "tensor_scalar", [b_t], [cbuf], out=kiT[:, sl], in0=xm[:], scalar1=kg[:, 0:1], scalar2=kg[:, 1:2], op0=ALU.mult, op1=ALU.add)
        fl = P.sbuf(s3, [128, 16, 16], F32, "fl")
        bfg = P.sbuf(s3, [128, 16], F32, "bfg")
        lf80 = P.sbuf(s3, [128, 16, 80], F32, "lf80")
        tri_i = P.sbuf(s3, [128, 128], F32, "tri_i")
        b_f = Buf()
        P.dma("sp", fl[:], flog_d.rearrange("(n p) h -> p n h", p=128), [], [b_f], "s3f")
        P.dma("sp", bfg[:], I("b_forget").partition_broadcast(128), [], [b_f], "s3fb")
        P.dma("sp", tri_i[:], I("triu"), [], [b_f], "s3ft")
        VOP("dve", "tensor_tensor", [b_f, cbuf], [b_f], out=tri_i[:], in0=tri_i[:], in1=ident_f[:], op=ALU.add)
        for n_ in range(16):
            VOP("dve", "tensor_tensor", [b_f], [b_f], out=fl[:, n_, :], in0=fl[:, n_, :], in1=bfg[:], op=ALU.add)
        ACT(fl[:], fl[:], AF.Exp, [b_f], [b_f], scale=-1.0)
        ACT(fl[:], fl[:], AF.Ln, [b_f], [b_f], bias=1.0)
        VOP("dve", "memset", [], [b_f], lf80[:], 0.0)
        for o_ in (0, 32, 64):
            VOP("dve", "tensor_scalar", [b_f], [b_f], out=lf80[:, :, o_:o_ + 16], in0=fl[:], scalar1=-1.0, scalar2=None, op0=ALU.mult)
        for j in range(16):
            out = PS[0:80, j * 128:(j + 1) * 128]
            for jp in range(j):
                MM(out, lf80[:, jp, :], ones_f[:], jp == 0, False, [b_f, cbuf], [pb[j // 4]])
            MM(out, lf80[:, j, :], tri_i[:], j == 0, True, [b_f], [pb[j // 4]])
        v32 = P.sbuf(s3, [80, S], F32, "v32")
        hf = P.sbuf(s3, [80, S], F32, "hf")
        hb = P.sbuf(s3, [80, S], BF16, "hb")
        b_c = Buf()
        VOP("dve", "tensor_scalar", [], [b_c] + pb[0:4], out=v32[:], in0=PS[0:80, 0:2048], scalar1=-1.0 / SCALE, scalar2=None, op0=ALU.mult)
        VOP("dve", "memset", [], [cbuf], CS[:], 0.0)
        for k_, o_ in enumerate((0, 32, 64)):
            VOP("dve", "tensor_copy", [b_c], [b_c], out=hb[:], in_=v32[:])
            VOP("dve", "tensor_copy", [b_c], [cbuf], out=CS[o_:o_ + 16, :], in_=hb[o_:o_ + 16, :])
            if k_ < 2:
                VOP("dve", "tensor_copy", [b_c], [b_c], out=hf[:], in_=hb[:])
                VOP("dve", "tensor_tensor", [b_c], [b_c], out=v32[:], in0=v32[:], in1=hf[:], op=ALU.subtract)
        P.barrier()
    if dbg and "cs_dbg" in dbg:
        dd = dscr("cs_dbg", [80, S], BF16)
        P.dma("sp", dd, CS[:], [], [], "dbg")
        dd2 = dscr("ki_dbg", [128, S], BF16)
        P.dma("sp", dd2, kiT[:], [], [], "dbg2")
        P.barrier()
    if stop_after <= 3:
        return nc, P

    with contextlib.ExitStack() as s5:
        cq = P.sbuf(s5, [128, 12, T], F32, "cq")
        cqn = P.sbuf(s5, [128, 12, T], BF16, "cqn")
        sq = P.sbuf(s5, [128, 12, 512], F32, "sq5")
        rstd = P.sbuf(s5, [128, 512], F32, "rstd5")
        qg = P.sbuf(s5, [128, 12], F32, "qg")
        wt = [P.sbuf(s5, [128, 12, 512], BF16, f"w5_{i}") for i in range(2)]
        wtb = bufs(2)
        stg = [P.sbuf(s5, [128, T], BF16, f"s5stg{i}") for i in range(2)]
        stgb = bufs(2)
        b_cq, b_sq, b_rs, b_n = Buf(), Buf(), Buf(), Buf()
        for r in range(12):
            P.dma("sp", cq[:, r, :], cqT_d[r], [], [b_cq], f"s5l{r % 2}")
        P.dma("sp", qg[:], I("qn_g"), [], [b_cq], "s5g")
        for tc in range(2):
            sl = slice(tc * 512, (tc + 1) * 512)
            for r in range(12):
                ACT(sq[:, r, :], cq[:, r, sl], AF.Square, [b_cq], [b_sq])
            for r in range(12):
                MM(ps(tc), ones_f[:], sq[:, r, :], r == 0, r == 11, [b_sq, cbuf], [pb[tc]])
            VOP("dve", "tensor_scalar", [], [b_rs, pb[tc]], out=rstd[:], in0=ps(tc), scalar1=1.0 / 1536, scalar2=1e-6, op0=ALU.mult, op1=ALU.add)
            RSQRT(rstd[:], b_rs)
            for r in range(12):
                VOP("dve", "scalar_tensor_tensor", [b_cq, b_rs], [b_n], out=cqn[:, r, sl], in0=cq[:, r, sl], scalar=qg[:, r:r + 1],
                    in1=rstd[:], op0=ALU.mult, op1=ALU.mult)
        n = 0
        for g in range(12):
            s = g % 2
            if g < 4:
                wsrc = I("w_uq").rearrange("(r p) n -> p r n", p=128)[:, :, g * 512:(g + 1) * 512]
            else:
                wsrc = I("w_iq").rearrange("(r p) n -> p r n", p=128)[:, :, (g - 4) * 512:(g - 3) * 512]
            cast_load(wt[s][:], wsrc, [], [wtb[s]], f"s5w{s}")
            for j in range(4):
                pr = n % 4
                n += 1
                for tc in range(2):
                    bank = pr * 2 + tc
                    for r in range(12):
                        MM(ps(bank), wt[s][:, r, j * 128:(j + 1) * 128], cqn[:, r, tc * 512:(tc + 1) * 512], r == 0, r == 11, [wtb[s], b_n], [pb[bank]])
                s2_ = n % 2
                evac(n, stg[s2_][:], PS[:, pr * 1024:(pr + 1) * 1024], [stgb[s2_], pb[pr * 2], pb[pr * 2 + 1]])
                dst = qdT_d[g * 4 + j] if g < 4 else qiT_d[(g - 4) * 4 + j]
                P.dma("sp", dst, stg[s2_][:], [stgb[s2_]], [], f"s5o{s2_}")
        P.barrier()
    if stop_after <= 5:
        return nc, P

    NIT = 22
    with contextlib.ExitStack() as s7:
        qi_t = [P.sbuf(s7, [128, 32, 128], BF16, f"qi{i}") for i in range(2)]
        qib = bufs(2)
        wi = P.sbuf(s7, [128, NQT, 32], F32, "wi")
        absw = P.sbuf(s7, [128, NQT, 32], F32, "absw")
        sgn = P.sbuf(s7, [128, NQT, 32], F32, "sgn")
        acc = [P.sbuf(s7, [128, S], F32, f"acc{i}") for i in range(1)]
        accb = bufs(2)
        rr = [P.sbuf(s7, [128, S], F32, f"rr{i}") for i in range(2)]
        rrb = bufs(2)
        junk = P.sbuf(s7, [128, S], BF16, "junk")
        selm = [P.sbuf(s7, [128, S], BF16, f"selm{i}") for i in range(2)]
        selb = bufs(2)
        sm = P.sbuf(s7, [128, 8], F32, "sm")
        b_w, b_s = Buf(), Buf()
        P.dma("sp", wi[:], widx_d.rearrange("(i p) h -> p i h", p=128), [], [b_w], "s7w")
        VOP("dve", "tensor_scalar", [b_w], [b_w], out=sgn[:], in0=wi[:], scalar1=-1.0, scalar2=None, op0=ALU.mult)
        VOP("dve", "tensor_tensor", [b_w], [b_w], out=absw[:], in0=wi[:], in1=sgn[:], op=ALU.max)
        VOP("dve", "tensor_scalar", [b_w], [b_w], out=absw[:], in0=absw[:], scalar1=1.0 / 64, scalar2=None, op0=ALU.mult)
        VOP("dve", "tensor_scalar", [b_w], [b_w], out=sgn[:], in0=wi[:], scalar1=0.0, scalar2=None, op0=ALU.is_ge)
        VOP("dve", "tensor_scalar", [b_w], [b_w], out=sgn[:], in0=sgn[:], scalar1=2.0, scalar2=-1.0, op0=ALU.mult, op1=ALU.add)
        VOP("dve", "memset", [], [b_s], sm[:, 6:7], 0.5)
        qsrc = qiT_d.rearrange("h p t -> p h t")
        mwt = [P.sbuf(s7, [128, KC, 512], BF16, f"mwt{i}") for i in range(3)]
        mwtb = bufs(3)
        mrow = [P.sbuf(s7, [1, 512], F32, f"mrow{i}") for i in range(3)]
        mrowb = bufs(3)
        mbrow = [P.sbuf(s7, [1, 512], F32, f"mbrow{i}") for i in range(3)]
        mbrowb = bufs(3)

        def mod_bg_compute(i_prev):
            for j in range(3):
                g = N_S0 + i_prev * 3 + j
                mod_compute(g, mwt[j], mwtb[j], mbrow[j], mbrowb[j], mrow[j], mrowb[j], 0, f"m{j}", False)

        for i in range(NQT):
            s = i % 2
            nk = 2 * i + 2
            ncol = nk * 128
            nch = (ncol + 511) // 512
            if i > 0:
                mod_bg_compute(i - 1)
            for j in range(3):
                g = N_S0 + i * 3 + j
                mod_issue(g, mwt[j], mwtb[j], mbrow[j], mbrowb[j], f"m{j}")
            P.dma("sp", qi_t[s][:], qsrc[:, :, i * 128:(i + 1) * 128], [], [qib[s]], f"s7q{s}")
            VOP("dve", "memset", [], [accb[0]], acc[0][:, 0:ncol], 0.0)
            VOP("dve", "tensor_copy", [cbuf], [accb[0]], out=acc[0][:, ncol - 256:ncol], in_=maskbig[:])
            for hi_ in range(32):
                sl = hi_ % 2
                for c in range(nch):
                    cw = min(512, ncol - c * 512)
                    bank = sl * 4 + c
                    MM(PS[:, bank * 512:bank * 512 + cw], qi_t[s][:, hi_, :], kiT[:, c * 512:c * 512 + cw], True, True, [qib[s], cbuf], [pb[bank]])
                ACT(rr[sl][:, 0:ncol], PS[:, sl * 2048:sl * 2048 + ncol], AF.Relu, [b_w], [rrb[sl]] + pb[sl * 4:sl * 4 + nch],
                    scale=absw[:, i, hi_:hi_ + 1])
                VOP("dve", "scalar_tensor_tensor", [rrb[sl], b_w], [accb[0]], out=acc[0][:, 0:ncol], in0=rr[sl][:, 0:ncol],
                    scalar=sgn[:, i, hi_:hi_ + 1], in1=acc[0][:, 0:ncol], op0=ALU.mult, op1=ALU.add)
            A = acc[0][:, 0:ncol]
            VOP("dve", "tensor_reduce", [accb[0]], [b_s], out=sm[:, 1:2], in_=A, axis=AX.X, op=ALU.max)
            VOP("dve", "tensor_scalar", [b_s], [b_s], out=sm[:, 0:1], in0=sm[:, 1:2], scalar1=-64.0, scalar2=None, op0=ALU.add)
            for it in range(NIT):
                hw_ = 32.0 / (2 ** it)
                VOP("dve", "tensor_scalar", [b_s], [b_s], out=sm[:, 2:3], in0=sm[:, 0:1], scalar1=hw_, scalar2=None, op0=ALU.add)
                VOP("dve", "memset", [], [b_s], sm[:, 3:4], 0.0)
                VOP("dve", "tensor_scalar", [accb[0], b_s], [b_s], out=junk[:, 0:ncol], in0=A, scalar1=sm[:, 2:3], scalar2=0.0, op0=ALU.is_ge, op1=ALU.add,
                    accum_out=sm[:, 3:4])
                VOP("dve", "tensor_scalar", [b_s], [b_s], out=sm[:, 4:5], in0=sm[:, 3:4], scalar1=255.5, scalar2=hw_, op0=ALU.is_ge, op1=ALU.mult)
                VOP("dve", "tensor_tensor", [b_s], [b_s], out=sm[:, 0:1], in0=sm[:, 0:1], in1=sm[:, 4:5], op=ALU.add)
            VOP("dve", "tensor_scalar", [accb[0], b_s], [selb[s]], out=selm[s][:, 0:ncol], in0=A, scalar1=sm[:, 0:1], scalar2=NEGBIG, op0=ALU.is_lt, op1=ALU.mult)
            P.dma("sp", sel_d[i][:, 0:ncol], selm[s][:, 0:ncol], [selb[s]], [], f"s7o{s}")
        mod_bg_compute(NQT - 1)
        P.barrier()
    if stop_after <= 7:
        return nc, P

    mT_d = dscr("mT_d", [KC, 128, T], BF16)
    sA = contextlib.ExitStack()
    attn = P.sbuf(sA, [128, NQT, D], BF16, "attn")
    ab = Buf("attn")
    with contextlib.ExitStack() as s8:
        kT_h = [P.sbuf(s8, [128, S], BF16, f"kTh{i}") for i in range(2)]
        qT_h = [P.sbuf(s8, [128, T], BF16, f"qTh{i}") for i in range(2)]
        v_h = [P.sbuf(s8, [128, 16, 132], BF16, f"vh{i}") for i in range(2)]
        G_h = [P.sbuf(s8, [128, 2304], BF16, f"Gh{i}") for i in range(2)]
        hb_ = bufs(2)
        Gf = [P.sbuf(s8, [128, 2304], F32, f"Gf{i}") for i in range(2)]
        gfb = bufs(2)
        selall = P.sbuf(s8, [128, NQT, S], BF16, "selall")
        b_sel = Buf()
        Pt = [P.sbuf(s8, [128, S], BF16, f"Pt{i}") for i in range(2)]
        ptb = bufs(2)
        PT = [P.sbuf(s8, [128, S], BF16, f"PT{i}") for i in range(2)]
        pTb = bufs(2)
        sm = P.sbuf(s8, [128, 8], F32, "sm8")
        smb = bufs(2)
        for s in range(2):
            VOP("dve", "memset", [], [hb_[s]], v_h[s][:, :, 128:132], 1.0)
        VOP("dve", "memset", [], [b_sel], selall[:], 0.0)
        for i in range(NQT):
            ncol = (2 * i + 2) * 128
            P.dma("sp", selall[:, i, 0:ncol], sel_d[i][:, 0:ncol], [], [b_sel], f"s8s{i % 2}")
        def head_loads(hd):
            fox = hd < 16
            h = hd % 16
            s = hd % 2
            P.dma("sp", kT_h[s][:], (kT_d if fox else kdT_d)[h], [], [hb_[s]], f"s8k{s}")
            P.dma("sp", qT_h[s][:], (qT_d if fox else qdT_d)[h], [], [hb_[s]], f"s8q{s}")
            vsrc = (v_d if fox else vd_d).rearrange("(n p) c -> p n c", p=128)[:, :, h * 128:(h + 1) * 128]
            P.dma("sp", v_h[s][:, :, 0:128], vsrc, [], [hb_[s]], f"s8v{s}")
            if not fox:
                P.dma("sp", Gf[s][:], I("biasG")[h], [], [gfb[s]], f"s8g{s}")
                VOP("dve", "tensor_scalar", [gfb[s]], [hb_[s]], out=G_h[s][:], in0=Gf[s][:], scalar1=1.0 / SCALE, scalar2=None, op0=ALU.mult)

        def stage_A(n):
            hd, i = divmod(n, NQT)
            fox = hd < 16
            h = hd % 16
            s = hd % 2
            s2 = n % 2
            nk = 2 * i + 2
            ncol = nk * 128
            nch = (ncol + 511) // 512
            qsl = qT_h[s][:, i * 128:(i + 1) * 128]
            for c in range(nch):
                cw = min(512, ncol - c * 512)
                out = PS[:, c * 512:c * 512 + cw]
                ksl = slice(c * 512, c * 512 + cw)
                MM(out, qsl, kT_h[s][:, ksl], True, False, [hb_[s]], [pb[c]])
                if fox:
                    if c == nch - 1:
                        MM(PS[:, ncol - 256:ncol], ident_b[:], maskb[:], False, False, [cbuf], [pb[c]])
                    MM(out, Esel[0:80, h * 128:(h + 1) * 128], CS[0:80, ksl], False, True, [cbuf], [pb[c]])
                else:
                    n0 = 2048 - 256 * i + c * 512
                    MM(out, ident_b[:], G_h[s][:, n0:n0 + cw], False, False, [cbuf, hb_[s]], [pb[c]])
                    MM(out, ident_b[:], selall[:, i, ksl], False, True, [cbuf, b_sel], [pb[c]])
            sc_ps = PS[:, 0:ncol]
            VOP("dve", "tensor_reduce", [], [smb[s2]] + pb[0:nch], out=sm[:, s2:s2 + 1], in_=sc_ps, axis=AX.X, op=ALU.max)
            VOP("dve", "tensor_scalar", [smb[s2]], [smb[s2]], out=sm[:, 2 + s2:3 + s2], in0=sm[:, s2:s2 + 1], scalar1=-SCALE, scalar2=None, op0=ALU.mult)
            ACT(Pt[s2][:, 0:ncol], sc_ps, AF.Exp, [smb[s2]], [ptb[s2]] + pb[0:nch], scale=SCALE, bias=sm[:, 2 + s2:3 + s2])

        def stage_B(n):
            hd, i = divmod(n, NQT)
            s2 = n % 2
            nk = 2 * i + 2
            ncol = nk * 128
            for kb in range(nk):
                TR(PSB[:, 4096 + kb * 128:4096 + (kb + 1) * 128], Pt[s2][:, kb * 128:(kb + 1) * 128], ident_b[:], [ptb[s2], cbuf], [pb[4 + kb // 8]])
            nb2 = (nk + 7) // 8
            evac(n, PT[s2][:, 0:ncol], PSB[:, 4096:4096 + ncol], [pTb[s2]] + pb[4:4 + nb2])

        def stage_C(n):
            hd, i = divmod(n, NQT)
            fox = hd < 16
            h = hd % 16
            s = hd % 2
            s2 = n % 2
            nk = 2 * i + 2
            ob = 6 + s2
            for kb in range(nk):
                MM(PS[:, ob * 512:ob * 512 + 129], PT[s2][:, kb * 128:(kb + 1) * 128], v_h[s][:, kb, 0:129], kb == 0, kb == nk - 1, [pTb[s2], hb_[s]], [pb[ob]])
            VOP("dve", "reciprocal", [], [smb2[s2], pb[ob]], out=sm[:, 4 + s2:5 + s2], in_=PS[:, ob * 512 + 128:ob * 512 + 129])
            col0 = (0 if fox else 2048) + h * 128
            ACT(attn[:, i, col0:col0 + 128], PS[:, ob * 512:ob * 512 + 128], AF.Copy, [smb2[s2]], [ab, pb[ob]], scale=sm[:, 4 + s2:5 + s2])

        smb2 = bufs(2)
        NIT8 = 32 * NQT
        for n in range(NIT8):
            if n % NQT == 0:
                head_loads(n // NQT)
            stage_A(n)
            if n > 0:
                stage_C(n - 1)
            stage_B(n)
        stage_C(NIT8 - 1)
        P.barrier()
    if dbg and "attn_d" in dbg:
        for i in range(NQT):
            P.dma("sp", attn_d[i * 128:(i + 1) * 128, :], attn[:, i, :], [], [], "dbg")
        P.barrier()
    if stop_after <= 8:
        return nc, P

    with contextlib.ExitStack() as s9:
        tmp = P.sbuf(s9, [128, 2048], F32, "tmp9")
        ssq = P.sbuf(s9, [128, 16], F32, "ssq")
        mg = P.sbuf(s9, [128, KC], F32, "mg")
        b_t, b_q = Buf(), Buf()
        P.dma("sp", mg[:], I("mg_g"), [], [b_q], "s9g")
        for i in range(NQT):
            for grp in range(2):
                seg = attn[:, i, grp * 2048:(grp + 1) * 2048]
                VOP("dve", "tensor_tensor", [ab], [b_t], out=tmp[:], in0=seg, in1=seg, op=ALU.mult)
                VOP("dve", "tensor_reduce", [b_t], [b_q], out=ssq[:, i * 2 + grp:i * 2 + grp + 1], in_=tmp[:], axis=AX.X, op=ALU.add)
        VOP("dve", "tensor_scalar", [b_q], [b_q], out=ssq[:], in0=ssq[:], scalar1=1.0 / 2048, scalar2=1e-6, op0=ALU.mult, op1=ALU.add)
        RSQRT(ssq[:], b_q)
        for i in range(NQT):
            for grp in range(2):
                seg = attn[:, i, grp * 2048:(grp + 1) * 2048]
                VOP("dve", "tensor_scalar", [b_q], [ab], out=seg, in0=seg, scalar1=ssq[:, i * 2 + grp:i * 2 + grp + 1], scalar2=None, op0=ALU.mult)
        n = 0
        mst = [P.sbuf(s9, [128, 8, 128], BF16, f"mst{i}") for i in range(4)]
        mstb = bufs(4)
        mTv = mT_d.rearrange("k p t -> p k t")
        for i in range(NQT):
            for k8 in range(4):
                bank = 4 + (n % 4)
                ms = n % 4
                n += 1
                for j in range(8):
                    kc = k8 * 8 + j
                    TR(PSB[:, bank * 1024 + j * 128:bank * 1024 + (j + 1) * 128], attn[:, i, kc * 128:(kc + 1) * 128], ident_b[:], [ab, cbuf], [pb[bank]])
                for j in range(8):
                    kc = k8 * 8 + j
                    src_ = PSB[:, bank * 1024 + j * 128:bank * 1024 + (j + 1) * 128]
                    dst = mst[ms][:, j, :]
                    if bank % 2 == 0:
                        ACT(dst, src_, AF.Copy, [b_q], [mstb[ms], pb[bank]], scale=mg[:, kc:kc + 1])
                    else:
                        VOP("dve", "tensor_scalar", [b_q], [mstb[ms], pb[bank]], out=dst, in0=src_, scalar1=mg[:, kc:kc + 1], scalar2=None, op0=ALU.mult)
                P.dma("sp", mTv[:, k8 * 8:(k8 + 1) * 8, i * 128:(i + 1) * 128], mst[ms][:], [mstb[ms]], [], f"s9m{ms}")
        P.barrier()
    sA.close()

    with contextlib.ExitStack() as s9:
        wt = [P.sbuf(s9, [128, KC, 512], BF16, f"wo{i}") for i in range(2)]
        wtb = bufs(2)
        GA = P.sbuf(s9, [128, D], F32, "GA")
        b_ga = Buf()
        xt = [P.sbuf(s9, [128, 512], F32, f"x9_{i}") for i in range(3)]
        xtb = bufs(3)
        t1 = [P.sbuf(s9, [128, 512], F32, f"t9_{i}") for i in range(2)]
        t1b = bufs(2)
        rt = [P.sbuf(s9, [128, 512], F32, f"r9_{i}") for i in range(2)]
        rtb = bufs(2)
        P.dma("sp", GA[:], mod_d[:, 2 * D:3 * D].partition_broadcast(128), [], [b_ga], "s9ga")
        mT = P.sbuf(s9, [128, KC, T], BF16, "mT")
        P.dma("sp", mT[:], mT_d.rearrange("k p t -> p k t"), [], [b_ga], "s9mt")
        wv = I("w_o").rearrange("(kc p) n -> p kc n", p=128)
        n = 0
        for dg in range(8):
            s = dg % 2
            dsl = slice(dg * 512, (dg + 1) * 512)
            cast_load(wt[s][:], wv[:, :, dsl], [], [wtb[s]], f"s9w{s}")
            for i in range(NQT):
                bank = n % 8
                s3_ = n % 3
                s2 = n % 2
                n += 1
                P.dma("sp", xt[s3_][:], I("x_own")[i * 128:(i + 1) * 128, dsl], [], [xtb[s3_]], f"s9x{s3_}")
                for kc in range(KC):
                    MM(ps(bank), mT[:, kc, i * 128:(i + 1) * 128], wt[s][:, kc, :], kc == 0, kc == KC - 1, [wtb[s], b_ga], [pb[bank]])
                VOP("dve", "tensor_tensor", [b_ga], [t1b[s2], pb[bank]], out=t1[s2][:], in0=ps(bank), in1=GA[:, dsl], op=ALU.mult)
                VOP("dve", "scalar_tensor_tensor", [xtb[s3_], t1b[s2]], [rtb[s2]], out=rt[s2][:], in0=xt[s3_][:], scalar=ALPHA, in1=t1[s2][:], op0=ALU.mult, op1=ALU.add)
                P.dma("sp", r_d[i * 128:(i + 1) * 128, dsl], rt[s2][:], [rtb[s2]], [], f"s9o{s2}")
        P.barrier()
    if stop_after <= 9:
        return nc, P

    sR = contextlib.ExitStack()
    gi = P.sbuf(sR, [128, 64], I32, "gi")
    sg = P.sbuf(sR, [128, 64], F32, "sg")
    rki = P.sbuf(sR, [128, NQT * 4], I32, "rki")
    b_rt = Buf("routing")
    sL = contextlib.ExitStack()
    LGT = P.sbuf(sL, [128, NQT, NE], F32, "LGT")
    with contextlib.ExitStack() as s10:
        LG = P.sbuf(s10, [128, D], F32, "LG")
        LB = P.sbuf(s10, [128, D], F32, "LB")
        A2 = P.sbuf(s10, [128, D], F32, "A2")
        B2 = P.sbuf(s10, [128, D], F32, "B2")
        rt = [P.sbuf(s10, [128, D], F32, f"rt{i}") for i in range(2)]
        rtb = bufs(2)
        tmp = P.sbuf(s10, [128, D], F32, "tmp10")
        u2f = P.sbuf(s10, [128, D], F32, "u2f")
        x1t = P.sbuf(s10, [128, D], F32, "x1t")
        u2b = P.sbuf(s10, [128, D], BF16, "u2b")
        u2T = P.sbuf(s10, [128, KC, 128], F32, "u2T")
        wr = P.sbuf(s10, [128, KC, NE], F32, "wr")
        brt = P.sbuf(s10, [128, NE], F32, "brt")
        st = P.sbuf(s10, [128, 4], F32, "st10")
        b_c, b_tmp, b_u, b_x1, b_ub, b_uT, b_st, b_lg = Buf(), Buf(), Buf(), Buf(), Buf(), Buf(), Buf(), Buf()
        P.dma("sp", LG[:], I("ln1_g").partition_broadcast(128), [], [b_c], "s10a")
        P.dma("sp", LB[:], I("ln1_b").partition_broadcast(128), [], [b_c], "s10b")
        P.dma("sp", tmp[:], mod_d[:, 4 * D:5 * D].partition_broadcast(128), [], [b_c], "s10a")
        P.dma("sp", u2f[:], mod_d[:, 3 * D:4 * D].partition_broadcast(128), [], [b_c], "s10b")
        P.dma("sp", wr[:], I("w_router").rearrange("(kc p) e -> p kc e", p=128), [], [b_c], "s10a")
        P.dma("sp", brt[:], I("b_router").partition_broadcast(128), [], [b_c], "s10b")
        VOP("dve", "scalar_tensor_tensor", [b_c], [b_c], out=A2[:], in0=tmp[:], scalar=1.0, in1=LG[:], op0=ALU.add, op1=ALU.mult)
        VOP("dve", "scalar_tensor_tensor", [b_c], [b_c], out=B2[:], in0=tmp[:], scalar=1.0, in1=LB[:], op0=ALU.add, op1=ALU.mult)
        VOP("dve", "tensor_tensor", [b_c], [b_c], out=B2[:], in0=B2[:], in1=u2f[:], op=ALU.add)
        P.barrier()
        for i in range(NQT):
            s = i % 2
            R = rt[s]
            rsl = slice(i * 128, (i + 1) * 128)
            P.dma("sp", R[:], r_d[rsl, :], [], [rtb[s]], f"s10r{s}")
            VOP("dve", "tensor_reduce", [rtb[s]], [b_st], out=st[:, 0:1], in_=R[:], axis=AX.X, op=ALU.add)
            VOP("dve", "tensor_scalar", [b_st], [b_st], out=st[:, 1:2], in0=st[:, 0:1], scalar1=-1.0 / D, scalar2=None, op0=ALU.mult)
            ACT(tmp[:], R[:], AF.Square, [rtb[s], b_st], [b_tmp], bias=st[:, 1:2])
            VOP("dve", "tensor_reduce", [b_tmp], [b_st], out=st[:, 2:3], in_=tmp[:], axis=AX.X, op=ALU.add)
            VOP("dve", "tensor_scalar", [b_st], [b_st], out=st[:, 2:3], in0=st[:, 2:3], scalar1=1.0 / D, scalar2=1e-5, op0=ALU.mult, op1=ALU.add)
            RSQRT(st[:, 2:3], b_st)
            VOP("dve", "tensor_scalar", [b_st], [rtb[s]], out=R[:], in0=R[:], scalar1=st[:, 1:2], scalar2=st[:, 2:3], op0=ALU.add, op1=ALU.mult)
            VOP("pool", "tensor_tensor", [rtb[s], b_c], [b_x1], out=x1t[:], in0=R[:], in1=LG[:], op=ALU.mult)
            VOP("pool", "tensor_tensor", [b_c], [b_x1], out=x1t[:], in0=x1t[:], in1=LB[:], op=ALU.add)
            P.dma("sp", x1_d[rsl, :], x1t[:], [b_x1], [], "s10x")
            VOP("dve", "tensor_tensor", [rtb[s], b_c], [b_u], out=u2f[:], in0=R[:], in1=A2[:], op=ALU.mult)
            VOP("dve", "tensor_tensor", [b_c], [b_u], out=u2f[:], in0=u2f[:], in1=B2[:], op=ALU.add)
            ACT(u2b[:], u2f[:], AF.Copy, [b_u], [b_ub])
            P.dma("sp", u2_d[rsl, :], u2b[:], [b_ub], [], "s10u")
            for k4 in range(8):
                bank = k4 % 4
                for j in range(4):
                    kc = k4 * 4 + j
                    TR(PS[:, bank * 512 + j * 128:bank * 512 + (j + 1) * 128], u2f[:, kc * 128:(kc + 1) * 128], ident_f[:], [b_u, cbuf], [pb[bank]])
                evac(bank, u2T[:, k4 * 4:(k4 + 1) * 4, :], PS[:, bank * 512:(bank + 1) * 512], [b_uT, pb[bank]])
            lb_ = 4 + (i % 2)
            for kc in range(KC):
                MM(PS[:, lb_ * 512:lb_ * 512 + NE], u2T[:, kc, :], wr[:, kc, :], kc == 0, kc == KC - 1, [b_uT, b_c], [pb[lb_]])
            VOP("dve", "tensor_tensor", [b_c], [b_lg, pb[lb_]], out=LGT[:, i, :], in0=PS[:, lb_ * 512:lb_ * 512 + NE], in1=brt[:], op=ALU.add)
        P.barrier()
    with contextlib.ExitStack() as sb:
        mx8 = P.sbuf(sb, [128, NQT, 8], F32, "mx8")
        idx8 = P.sbuf(sb, [128, NQT, 8], U32, "idx8")
        idxf = P.sbuf(sb, [128, NQT, 8], F32, "idxf")
        MK = P.sbuf(sb, [128, NQT, NE], F32, "MK")
        GT = P.sbuf(sb, [128, NQT, NE], F32, "GT")
        MKb = P.sbuf(sb, [128, NQT, NE], BF16, "MKb")
        POS = P.sbuf(sb, [128, NQT, NE], F32, "POS")
        RF = P.sbuf(sb, [128, NQT, NE], F32, "RF")
        VL = P.sbuf(sb, [128, NQT, NE], F32, "VL")
        iot = P.sbuf(sb, [128, 256], F32, "iot")
        tix = P.sbuf(sb, [128, 16], F32, "tix")
        trs = P.sbuf(sb, [128, 128], F32, "trs")
        trsb = P.sbuf(sb, [128, 128], BF16, "trsb")
        ecol = P.sbuf(sb, [128, NE], F32, "ecol")
        s1 = P.sbuf(sb, [128, 16], F32, "s1")
        oh = P.sbuf(sb, [128, NE], F32, "oh")
        rkf = P.sbuf(sb, [128, NQT * 4], F32, "rkf")
        Se = [P.sbuf(sb, [128, NQT, CAP], BF16, f"Se{i}") for i in range(2)]
        R5 = P.sbuf(sb, [128, NQT, NE, 5], BF16, "R5")
        gtmp = P.sbuf(sb, [128, NQT, NE], F32, "gtmp")
        gres = P.sbuf(sb, [128, NQT, NE], F32, "gres")
        Seb = bufs(2)
        SI = P.sbuf(sb, [128, 64, 5], F32, "SI")
        vtmp = P.sbuf(sb, [128, 64], F32, "vtmp")
        gf = P.sbuf(sb, [128, 64], F32, "gf")
        b_k = Buf()
        P.dma("sp", iot[:], I("iota"), [], [b_k], "s10a")
        P.dma("sp", tix[:], I("tidx"), [], [b_k], "s10b")
        P.dma("sp", trs[:], I("triu"), [], [b_k], "s10a")
        VOP("dve", "tensor_copy", [b_k], [b_k], out=trsb[:], in_=trs[:])
        VOP("dve", "tensor_scalar", [b_k], [b_k], out=ecol[:], in0=iot[:, 0:NE], scalar1=float(CAP), scalar2=None, op0=ALU.mult)
        for i in range(NQT):
            VOP("dve", "max", [b_lg], [b_k], out=mx8[:, i, :], in_=LGT[:, i, :])
            VOP("dve", "max_index", [b_lg, b_k], [b_k], out=idx8[:, i, :], in_max=mx8[:, i, :], in_values=LGT[:, i, :])
            VOP("dve", "tensor_scalar", [b_lg, b_k], [b_k], out=MK[:, i, :], in0=LGT[:, i, :], scalar1=mx8[:, i, 3:4], scalar2=None, op0=ALU.is_ge)
            VOP("dve", "tensor_scalar", [b_k], [b_k], out=s1[:, i:i + 1], in0=mx8[:, i, 0:1], scalar1=-1.0, scalar2=None, op0=ALU.mult)
            ACT(GT[:, i, :], LGT[:, i, :], AF.Exp, [b_lg, b_k], [b_k], bias=s1[:, i:i + 1])
            VOP("dve", "tensor_tensor", [b_k], [b_k], out=GT[:, i, :], in0=GT[:, i, :], in1=MK[:, i, :], op=ALU.mult)
            VOP("dve", "tensor_reduce", [b_k], [b_k], out=s1[:, 8 + i:9 + i], in_=GT[:, i, :], axis=AX.X, op=ALU.add)
        VOP("dve", "reciprocal", [b_k], [b_k], out=s1[:, 8:16], in_=s1[:, 8:16])
        for i in range(NQT):
            VOP("dve", "tensor_scalar", [b_k], [b_k], out=GT[:, i, :], in0=GT[:, i, :], scalar1=s1[:, 8 + i:9 + i], scalar2=None, op0=ALU.mult)
        VOP("dve", "tensor_copy", [b_k], [b_k], out=idxf[:], in_=idx8[:])
        VOP("dve", "tensor_copy", [b_k], [b_k], out=MKb[:], in_=MK[:])
        for i in range(NQT):
            out = PS[:, i * NE:(i + 1) * NE]
            for ip in range(i):
                MM(out, ones_b[:], MKb[:, ip, :], ip == 0, False, [b_k, cbuf], [pb[0]])
            MM(out, trsb[:], MKb[:, i, :], i == 0, True, [b_k], [pb[0]])
        VOP("dve", "tensor_copy", [], [b_k, pb[0]], out=POS[:].rearrange("p a b -> p (a b)"), in_=PS[:, 0:NQT * NE])
        for i in range(NQT):
            VOP("dve", "tensor_tensor", [b_k], [b_k], out=RF[:, i, :], in0=POS[:, i, :], in1=ecol[:], op=ALU.add)
        VOP("dve", "tensor_scalar", [b_k], [b_k], out=RF[:], in0=RF[:], scalar1=-float(ZROW), scalar2=None, op0=ALU.add)
        VOP("dve", "tensor_scalar", [b_k], [b_k], out=VL[:], in0=POS[:], scalar1=CAP - 0.5, scalar2=None, op0=ALU.is_lt)
        VOP("dve", "tensor_tensor", [b_k], [b_k], out=RF[:], in0=RF[:], in1=VL[:], op=ALU.mult)
        VOP("dve", "tensor_scalar", [b_k], [b_k], out=RF[:], in0=RF[:], scalar1=float(ZROW), scalar2=None, op0=ALU.add)
        for i in range(NQT):
            for k in range(4):
                VOP("dve", "tensor_scalar", [b_k], [b_k], out=oh[:], in0=iot[:, 0:NE], scalar1=idxf[:, i, k:k + 1], scalar2=None, op0=ALU.is_equal)
                VOP("dve", "tensor_tensor", [b_k], [b_k], out=oh[:], in0=oh[:], in1=RF[:, i, :], op=ALU.mult)
                VOP("dve", "tensor_reduce", [b_k], [b_k], out=rkf[:, i * 4 + k:i * 4 + k + 1], in_=oh[:], axis=AX.X, op=ALU.add)
        VOP("dve", "tensor_copy", [b_k], [b_rt], out=rki[:], in_=rkf[:])
        for i in range(NQT):
            for k in range(2):
                VOP("dve", "tensor_scalar", [b_k, cbuf], [b_k], out=R5[:, i, :, k], in0=ones_f[:, 0:NE], scalar1=tix[:, k * 8 + i:k * 8 + i + 1], scalar2=None, op0=ALU.mult)
        VOP("dve", "tensor_copy", [b_k], [b_k], out=gres[:], in_=GT[:])
        for k in range(3):
            VOP("dve", "tensor_copy", [b_k], [b_k], out=R5[:, :, :, 2 + k], in_=gres[:])
            if k < 2:
                VOP("dve", "tensor_copy", [b_k], [b_k], out=gtmp[:], in_=R5[:, :, :, 2 + k])
                VOP("dve", "tensor_tensor", [b_k], [b_k], out=gres[:], in0=gres[:], in1=gtmp[:], op=ALU.subtract)
        for e in range(NE):
            s = e % 2
            for i in range(NQT):
                VOP("dve", "tensor_scalar", [b_k], [Seb[s]], out=Se[s][:, i, :], in0=iot[:, 0:CAP], scalar1=POS[:, i, e:e + 1], scalar2=MK[:, i, e:e + 1],
                    op0=ALU.is_equal, op1=ALU.mult)
            for b in range(2):
                col = 512 + (e * 2 + b) * 5
                for i in range(NQT):
                    MM(PS[:, col:col + 5], Se[s][:, i, b * 128:(b + 1) * 128], R5[:, i, e, :], i == 0, i == NQT - 1, [Seb[s], b_k], [pb[1]])
        VOP("dve", "tensor_copy", [], [b_k, pb[1]], out=SI[:].rearrange("p a b -> p (a b)"), in_=PS[:, 512:512 + 320])
        VOP("dve", "scalar_tensor_tensor", [b_k], [b_k], out=vtmp[:], in0=SI[:, :, 0], scalar=32.0, in1=SI[:, :, 1], op0=ALU.mult, op1=ALU.add)
        VOP("dve", "tensor_scalar", [b_k], [b_k], out=gf[:], in0=vtmp[:], scalar1=-1.0, scalar2=0.0, op0=ALU.add, op1=ALU.max)
        VOP("dve", "tensor_copy", [b_k], [b_rt], out=gi[:], in_=gf[:])
        VOP("dve", "tensor_tensor", [b_k], [b_k], out=vtmp[:], in0=SI[:, :, 2], in1=SI[:, :, 3], op=ALU.add)
        VOP("dve", "tensor_tensor", [b_k], [b_rt], out=sg[:], in0=vtmp[:], in1=SI[:, :, 4], op=ALU.add)
        P.barrier()
    sL.close()
    if dbg and "rt_dbg" in dbg:
        d1_ = dscr("rt_dbg", [128, 64 + 64 + 32], F32)
        with contextlib.ExitStack() as sd:
            tdb = P.sbuf(sd, [128, 160], F32, "tdb")
            b_d = Buf()
            VOP("dve", "tensor_copy", [b_rt], [b_d], out=tdb[:, 0:64], in_=gi[:])
            VOP("dve", "tensor_copy", [b_rt], [b_d], out=tdb[:, 64:128], in_=sg[:])
            VOP("dve", "tensor_copy", [b_rt], [b_d], out=tdb[:, 128:160], in_=rki[:])
            P.dma("sp", d1_, tdb[:], [b_d], [], "dbg")
            P.barrier()
    if stop_after <= 10:
        return nc, P

    W1COLS, W2COLS = 3072, D
    with contextlib.ExitStack() as s11:
        W1G = 384
        wt1 = [P.sbuf(s11, [128, KC, W1G], BF16, f"w1_{i}") for i in range(3)]
        wt1b = bufs(3)
        wt2 = [P.sbuf(s11, [128, 12, 512], BF16, f"w2_{i}") for i in range(2)]
        wt2b = bufs(2)
        xg = [P.sbuf(s11, [128, D], BF16, f"xg{i}") for i in range(2)]
        xgb = bufs(2)
        xgT = [P.sbuf(s11, [128, KC, 128], BF16, f"xgT{i}") for i in range(2)]
        xgTb = bufs(2)
        B1bc = P.sbuf(s11, [128, W1COLS], F32, "B1bc")
        B2bc = P.sbuf(s11, [128, D], F32, "B2bc")
        b_b1, b_b2 = Buf(), Buf()
        gact = [P.sbuf(s11, [128, 1536], F32, f"gact{i}") for i in range(2)]
        gab = bufs(2)
        act = [P.sbuf(s11, [128, 1536], BF16, f"act{i}") for i in range(2)]
        actb = bufs(2)
        actT = [P.sbuf(s11, [128, 12, 128], BF16, f"actT{i}") for i in range(2)]
        actTb = bufs(2)
        ot = [P.sbuf(s11, [128, 512], F32, f"ot{i}") for i in range(2)]
        otb = bufs(2)
        tq = [P.sbuf(s11, [128, 512], F32, f"tq{i}") for i in range(2)]
        tqb = bufs(2)
        sgm = [P.sbuf(s11, [128, 512], F32, f"sgm{i}") for i in range(1)]
        sgmb = bufs(1)
        VOP("dve", "memset", [], [otb[0]], ot[0][:], 0.0)
        for q4 in range(8):
            P.dma("sp", ys_d[ZROW:ZROW + 128, q4 * 512:(q4 + 1) * 512], ot[0][:], [otb[0]], [], "s11z")
        groups = []
        for e in range(ne):
            for g in range(8):
                groups.append((1, e, g))
            for g in range(8):
                groups.append((2, e, g))

        def issue_load(n):
            kind, e, g = groups[n]
            if kind == 1:
                s = (e * 8 + g) % 3
                cast_load(wt1[s][:], I("w1")[e].rearrange("(kc p) n -> p kc n", p=128)[:, :, g * W1G:(g + 1) * W1G], [], [wt1b[s]], f"s11w1{s}")
            else:
                s = g % 2
                cast_load(wt2[s][:], I("w2")[e].rearrange("(kc p) n -> p kc n", p=128)[:, :, g * 512:(g + 1) * 512], [], [wt2b[s]], f"s11w2{s}")

        def pe_fill(k):
            for _ in range(k):
                MM(PS[:, 4 * 512:5 * 512], ident_b[:], kiT[:, 0:512], True, True, [cbuf], [pb[4]])

        issued = set()

        def maybe_load(m, kinds):
            if m < len(groups) and m not in issued and groups[m][0] in kinds:
                issued.add(m)
                issue_load(m)

        maybe_load(0, (1, 2))
        maybe_load(1, (1,))
        nb = 0
        nt = 0
        for n, (kind, e, g) in enumerate(groups):
            if kind == 1 and g == 0:
                P.dma("sp", B1bc[:], I("b1")[e:e + 1, :].partition_broadcast(128), [], [b_b1], "s11b1")
                P.dma("sp", B2bc[:], I("b2")[e:e + 1, :].partition_broadcast(128), [], [b_b2], "s11b2")
                for b in range(2):
                    k = e * 2 + b
                    P.op("pool", lambda en, b=b, k=k: en.indirect_dma_start(out=xg[b][:, :], out_offset=None, in_=u2_d[:, :],
                         in_offset=bass.IndirectOffsetOnAxis(ap=gi[:, k:k + 1], axis=0), bounds_check=None),
                         [b_rt], [xgb[b]], dma=True, chan=f"s11g{b}")
            maybe_load(n + 1, (1, 2))
            maybe_load(n + 2, (1,))
            if kind == 1 and g == 0:
                for b in range(2):
                    for f2 in range(2):
                        for j in range(16):
                            kc = f2 * 16 + j
                            TR(PSB[:, 4096 + j * 128:4096 + (j + 1) * 128], xg[b][:, kc * 128:(kc + 1) * 128], ident_b[:], [xgb[b], cbuf], [pb[4 + j // 8]])
                        evac(f2, xgT[b][:, f2 * 16:(f2 + 1) * 16, :], PSB[:, 4096:4096 + 2048], [xgTb[b], pb[4], pb[5]])
            if kind == 1:
                s = (e * 8 + g) % 3
                csl = slice(g * W1G, (g + 1) * W1G)
                for b in range(2):
                    bank = nb % 4
                    nb += 1
                    tqi = nt % 2
                    nt += 1
                    pso = PS[:, bank * 512:bank * 512 + W1G]
                    tqv = tq[tqi][:, 0:W1G]
                    for kc in range(KC):
                        MM(pso, xgT[b][:, kc, :], wt1[s][:, kc, :], kc == 0, kc == KC - 1, [xgTb[b], wt1b[s]], [pb[bank]])
                    VOP("dve", "tensor_tensor", [b_b1], [tqb[tqi], pb[bank]], out=tqv, in0=pso, in1=B1bc[:, csl], op=ALU.add)
                    if g < 4:
                        VOP("dve", "tensor_scalar", [tqb[tqi]], [tqb[tqi]], out=tqv, in0=tqv, scalar1=7.0, scalar2=None, op0=ALU.min)
                        sgi = 0
                        ACT(sgm[sgi][:, 0:W1G], tqv, AF.Sigmoid, [tqb[tqi]], [sgmb[sgi]], scale=1.702)
                        VOP("dve", "tensor_tensor", [tqb[tqi], sgmb[sgi]], [gab[b]], out=gact[b][:, csl], in0=tqv, in1=sgm[sgi][:, 0:W1G], op=ALU.mult)
                    else:
                        c2 = slice((g - 4) * W1G, (g - 3) * W1G)
                        VOP("dve", "tensor_scalar", [tqb[tqi]], [tqb[tqi]], out=tqv, in0=tqv, scalar1=7.0, scalar2=-7.0, op0=ALU.min, op1=ALU.max)
                        VOP("dve", "scalar_tensor_tensor", [tqb[tqi], gab[b]], [actb[b]], out=act[b][:, c2], in0=tqv, scalar=1.0, in1=gact[b][:, c2],
                            op0=ALU.add, op1=ALU.mult)
                pe_fill(20)
                if g == 7:
                    for b in range(2):
                        for j in range(12):
                            TR(PSB[:, 6144 + j * 128:6144 + (j + 1) * 128], act[b][:, j * 128:(j + 1) * 128], ident_b[:], [actb[b], cbuf], [pb[6 + j // 8]])
                        evac(b, actT[b][:].rearrange("p a b -> p (a b)"), PSB[:, 6144:6144 + 1536], [actTb[b], pb[6], pb[7]])
            else:
                s = g % 2
                csl = slice(g * 512, (g + 1) * 512)
                for b in range(2):
                    bank = nb % 4
                    nb += 1
                    tqi = nt % 2
                    nt += 1
                    for kc in range(12):
                        MM(ps(bank), actT[b][:, kc, :], wt2[s][:, kc, :], kc == 0, kc == 11, [actTb[b], wt2b[s]], [pb[bank]])
                    VOP("dve", "tensor_tensor", [b_b2], [tqb[tqi], pb[bank]], out=tq[tqi][:], in0=ps(bank), in1=B2bc[:, csl], op=ALU.add)
                    k = e * 2 + b
                    ACT(ot[b][:], tq[tqi][:], AF.Copy, [tqb[tqi], b_rt], [otb[b]], scale=sg[:, k:k + 1])
                    P.dma("sp", ys_d[e * CAP + b * 128:e * CAP + (b + 1) * 128, g * 512:(g + 1) * 512], ot[b][:], [otb[b]], [], f"s11o{b}")
                pe_fill(10)
        P.barrier()
    if stop_after <= 11:
        return nc, P

    with contextlib.ExitStack() as s12:
        GM = P.sbuf(s12, [128, D], F32, "GM")
        L2G = P.sbuf(s12, [128, D], F32, "L2G")
        L2B = P.sbuf(s12, [128, D], F32, "L2B")
        yk = [P.sbuf(s12, [128, D], F32, f"yk{i}") for i in range(4)]
        ykb = bufs(4)
        x1t = P.sbuf(s12, [128, D], F32, "x1t12")
        tmp = P.sbuf(s12, [128, D], F32, "tmp12")
        st = P.sbuf(s12, [128, 4], F32, "st12")
        b_c, b_x, b_t, b_st = Buf(), Buf(), Buf(), Buf()
        P.dma("sp", GM[:], mod_d[:, 5 * D:6 * D].partition_broadcast(128), [], [b_c], "s12a")
        P.dma("sp", L2G[:], I("ln2_g").partition_broadcast(128), [], [b_c], "s12b")
        P.dma("sp", L2B[:], I("ln2_b").partition_broadcast(128), [], [b_c], "s12a")
        outs = []
        for i in range(NQT):
            rsl = slice(i * 128, (i + 1) * 128)
            P.dma("sp", x1t[:], x1_d[rsl, :], [], [b_x], "s12x")
            for k in range(4):
                c = i * 4 + k
                P.op("pool", lambda en, k=k, c=c: en.indirect_dma_start(out=yk[k][:, :], out_offset=None, in_=ys_d[:, :],
                     in_offset=bass.IndirectOffsetOnAxis(ap=rki[:, c:c + 1], axis=0), bounds_check=None),
                     [b_rt], [ykb[k]], dma=True, chan=f"s12g{k}")
            VOP("dve", "tensor_tensor", [ykb[0], ykb[1]], [ykb[0]], out=yk[0][:], in0=yk[0][:], in1=yk[1][:], op=ALU.add)
            VOP("dve", "tensor_tensor", [ykb[2], ykb[3]], [ykb[2]], out=yk[2][:], in0=yk[2][:], in1=yk[3][:], op=ALU.add)
            VOP("dve", "tensor_tensor", [ykb[0], ykb[2]], [ykb[0]], out=yk[0][:], in0=yk[0][:], in1=yk[2][:], op=ALU.add)
            VOP("dve", "tensor_tensor", [ykb[0], b_c], [ykb[0]], out=yk[0][:], in0=yk[0][:], in1=GM[:], op=ALU.mult)
            VOP("dve", "scalar_tensor_tensor", [b_x, ykb[0]], [b_x], out=x1t[:], in0=x1t[:], scalar=ALPHA, in1=yk[0][:], op0=ALU.mult, op1=ALU.add)
            VOP("dve", "tensor_reduce", [b_x], [b_st], out=st[:, 0:1], in_=x1t[:], axis=AX.X, op=ALU.add)
            VOP("dve", "tensor_scalar", [b_st], [b_st], out=st[:, 1:2], in0=st[:, 0:1], scalar1=-1.0 / D, scalar2=None, op0=ALU.mult)
            ACT(tmp[:], x1t[:], AF.Square, [b_x, b_st], [b_t], bias=st[:, 1:2])
            VOP("dve", "tensor_reduce", [b_t], [b_st], out=st[:, 2:3], in_=tmp[:], axis=AX.X, op=ALU.add)
            VOP("dve", "tensor_scalar", [b_st], [b_st], out=st[:, 2:3], in0=st[:, 2:3], scalar1=1.0 / D, scalar2=1e-5, op0=ALU.mult, op1=ALU.add)
            RSQRT(st[:, 2:3], b_st)
            VOP("dve", "tensor_scalar", [b_x, b_st], [b_t], out=tmp[:], in0=x1t[:], scalar1=st[:, 1:2], scalar2=st[:, 2:3], op0=ALU.add, op1=ALU.mult)
            VOP("dve", "tensor_tensor", [b_c], [b_t], out=tmp[:], in0=tmp[:], in1=L2G[:], op=ALU.mult)
            VOP("dve", "tensor_tensor", [b_c], [b_t], out=tmp[:], in0=tmp[:], in1=L2B[:], op=ALU.add)
            outs.append(P.dma("sp", y_out[rsl, :], tmp[:], [b_t], [], "s12o"))
        P.barrier()
    sR.close()
    return nc, P


def _t5_bucket(dist):
    import math
    d = np.maximum(dist.astype(np.float32), np.float32(1.0))
    large = 16 + (np.log(d / np.float32(16)) / np.float32(math.log(128 / 16)) * np.float32(16)).astype(np.int32)
    large = np.minimum(large, 31)
    return np.where(dist < 16, dist, large)


def _fm(v, n):
    return np.ascontiguousarray(np.asarray(v, np.float32).reshape(n, 128).T)


def prep_core(inp, core, ne=NE):
    b, par = core // 2, core % 2
    f = lambda a: np.ascontiguousarray(np.asarray(a, np.float32))
    x = f(inp["x"][b])
    own = np.concatenate([x[(2 * i + par) * 128:(2 * i + par + 1) * 128] for i in range(NQT)], axis=0)
    p = np.arange(128)[:, None]
    n = np.arange(2304)[None, :]
    dist = np.maximum(par * 128 + p + 2048 - n, 0)
    bidx = _t5_bucket(dist)
    rb = f(inp["rel_bias"])
    biasG = np.ascontiguousarray(np.transpose(rb[bidx], (2, 0, 1)))
    esel = np.zeros((80, 16, 128), np.float32)
    for h in range(16):
        esel[h, h, :] = 1; esel[32 + h, h, :] = 1; esel[64 + h, h, :] = 1
    masked = (np.arange(128)[None, :] > np.arange(128)[:, None]).astype(np.float32)
    if par == 0:
        maskc = np.concatenate([masked, np.ones((128, 128), np.float32)], axis=1)
    else:
        maskc = np.concatenate([np.zeros((128, 128), np.float32), masked], axis=1)
    m = {
        "x_all": x, "x_own": f(own), "cT": _fm(inp["c"][b], KC),
        "w_ada": f(inp["w_ada"][0]), "b_ada": f(inp["b_ada"][0]).reshape(1, -1),
        "w_in": f(inp["w_in"][0]), "b_forget": f(inp["b_forget"][0]).reshape(1, 16),
        "qn_g": _fm(inp["q_norm_g"][0], 12), "kvn_g": _fm(inp["kv_norm_g"][0], 4),
        "ki_gb": np.ascontiguousarray(np.stack([f(inp["kidx_ln_g"][0]), f(inp["kidx_ln_b"][0])], axis=1)),
        "w_uq": f(inp["w_uq"][0]), "w_uk": f(inp["w_uk"][0]).reshape(512, 2048), "w_uv": f(inp["w_uv"][0]).reshape(512, 2048),
        "w_iq": f(inp["w_iq"][0]),
        "mg_g": _fm(np.concatenate([f(inp["fox_out_g"][0]), f(inp["dsa_out_g"][0])]), KC),
        "w_o": f(inp["w_o"][0]), "ln1_g": f(inp["ln1_g"][0]).reshape(1, -1), "ln1_b": f(inp["ln1_b"][0]).reshape(1, -1),
        "w_router": f(inp["w_router"][0]), "b_router": f(inp["b_router"][0]).reshape(1, -1),
        "w1": f(inp["w1"][0][:ne]), "b1": f(inp["b1"][0][:ne]), "w2": f(inp["w2"][0][:ne]), "b2": f(inp["b2"][0][:ne]),
        "ln2_g": f(inp["ln2_g"][0]).reshape(1, -1), "ln2_b": f(inp["ln2_b"][0]).reshape(1, -1),
        "biasG": biasG, "ident": np.eye(128, dtype=np.float32), "esel": esel.reshape(80, 2048),
        "maskc": maskc, "triu": (np.arange(128)[:, None] < np.arange(128)[None, :]).astype(np.float32),
        "iota": np.tile(np.arange(256, dtype=np.float32)[None, :], (128, 1)),
        "tidx": np.concatenate([(np.arange(8)[None, :] * 128 + np.arange(128)[:, None] + 1) // 32,
                                (np.arange(8)[None, :] * 128 + np.arange(128)[:, None] + 1) % 32], axis=1).astype(np.float32),
    }
    return m


def kernel(**inputs):
    nc, P = build()
    P.emit()
    n = 8
    in_maps = [prep_core(inputs, c) for c in range(n)]
    res = run_bass_kernel_spmd(nc, in_maps, core_ids=list(range(n)))
    out = np.empty((4, S, D), np.float32)
    for c in range(n):
        b, par = c // 2, c % 2
        y = np.asarray(res.results[c]["y"])
        for i in range(NQT):
            out[b, (2 * i + par) * 128:(2 * i + par + 1) * 128, :] = y[i * 128:(i + 1) * 128, :]
    return out
```
